# Optimizing a Trainium2 kernel written in Bass

```python
import math
import jax, jax.numpy as jnp
from jax import lax
import numpy as np

D_MODEL = 2048
BATCH = 4
SEQ = 4096
DEPTH = 2
DEC_BATCH = 32
DEC_SEQ = 64
PAST_LEN = 1024

CHUNK = 64
Q_BLOCK = 128
D_Q_BLOCK = 64
EPS = 1e-6
NEG_INF = -1e30
H_A = 8
DH_A = 64
A_BAND_CHUNKS = 8
A_WINDOW = A_BAND_CHUNKS * CHUNK
A_CLIP = 256
H_B = 4
DH_B = 64
H_C = 4
C_NOPE = 64
C_ROPE = 32
C_V = 128
C_Q_RANK = 384
C_KV_RANK = 256
ROPE_THETA = 10000.0
H_D = 8
DH_D = 64
IDX_HEADS = 8
IDX_DIM = 64
IDX_TOPK = 256
T5_BUCKETS = 32
T5_MAX_DIST = 128
N_BRANCH = 4
BRANCH_W = 512
D_FF = 5632
N_EXPERTS = 8
TOP_K = 2
D_FF_E = 7168
IN_SIZES = ((H_A * DH_A,) * 3 + (H_B * 2 * DH_B,) * 3 + (C_Q_RANK, C_KV_RANK, C_ROPE)
            + (H_D * DH_D,) * 3 + (IDX_HEADS * IDX_DIM, IDX_DIM, IDX_HEADS))
IN_COLS = sum(IN_SIZES)

kernel_name = 'chunk_causal_hybrid_encoder_step'


def rmsnorm(x, g):
    xf = x.astype(jnp.float32)
    y = xf * lax.rsqrt(jnp.mean(xf * xf, axis=-1, keepdims=True) + EPS)
    return (y * g.astype(jnp.float32)).astype(x.dtype)


def masked_softmax(logits, mask):
    return jax.nn.softmax(jnp.where(mask, logits.astype(jnp.float32), NEG_INF), axis=-1)


def chunk_causal(qpos, kpos):
    return (kpos[None, :] // CHUNK) <= (qpos[:, None] // CHUNK)


def t5_bucket(rel):
    nb = T5_BUCKETS // 2
    max_exact = nb // 2
    ret = jnp.where(rel > 0, nb, 0)
    n = jnp.abs(rel)
    nf = jnp.maximum(n, 1).astype(jnp.float32)
    large = max_exact + (jnp.log(nf / max_exact) / math.log(T5_MAX_DIST / max_exact)
                         * (nb - max_exact)).astype(jnp.int32)
    large = jnp.minimum(large, nb - 1)
    return ret + jnp.where(n < max_exact, n, large)


def t5_rel_bias(qpos, kpos, tab):
    return jnp.transpose(tab[t5_bucket(kpos[None, :] - qpos[:, None])], (2, 0, 1))


def apply_rope(x, pos):
    half = x.shape[-1] // 2
    freqs = ROPE_THETA ** (-jnp.arange(half, dtype=jnp.float32) / half)
    ang = pos.astype(jnp.float32)[:, None] * freqs
    shape = (pos.shape[0],) + (1,) * (x.ndim - 3) + (half,)
    cos = jnp.cos(ang).reshape(shape)
    sin = jnp.sin(ang).reshape(shape)
    xf = x.astype(jnp.float32)
    x1, x2 = xf[..., :half], xf[..., half:]
    return jnp.concatenate([x1 * cos - x2 * sin, x2 * cos + x1 * sin], axis=-1).astype(x.dtype)


def over_query_blocks(fn, qs, qpos, block):
    T = qpos.shape[0]
    if T <= block:
        return fn(qs, qpos)
    nb = T // block
    qs_b = tuple(jnp.moveaxis(q.reshape(q.shape[0], nb, block, *q.shape[2:]), 1, 0) for q in qs)
    out = lax.map(lambda a: fn(a[0], a[1]), (qs_b, qpos.reshape(nb, block)))
    out = jnp.moveaxis(out, 0, 1)
    return out.reshape(out.shape[0], T, *out.shape[3:])


def band_attention(q, k, v, qpos, kpos, rel_table):
    qc = (qpos // CHUNK)[:, :, None]
    kc = (kpos // CHUNK)[:, None, :]
    mask = (kc <= qc) & (kc >= qc - A_BAND_CHUNKS) & (kpos[:, None, :] >= 0)
    rel = jnp.clip(kpos[:, None, :] - qpos[:, :, None], -A_CLIP, A_CLIP) + A_CLIP
    bias = jnp.moveaxis(rel_table[rel], -1, 1)
    logits = jnp.einsum('bnqhd,bnkhd->bnhqk', q, k) * DH_A ** -0.5 + bias
    p = masked_softmax(logits, mask[:, None])
    return jnp.einsum('bnhqk,bnkhd->bnqhd', p.astype(v.dtype), v)


def diff_core(q, qpos, k, v, kpos, tab, lam):
    bias = t5_rel_bias(qpos, kpos, tab)
    logits = jnp.einsum('bqhjd,bkhjd->bjhqk', q, k) * DH_B ** -0.5 + bias
    p = masked_softmax(logits, chunk_causal(qpos, kpos))
    attn = p[:, 0] - lam * p[:, 1]
    return jnp.einsum('bhqk,bkhe->bqhe', attn.astype(v.dtype), v)


def mla_core(q_lat, q_rope, qpos, lat, kr, kpos):
    logits = (jnp.einsum('bqhr,bkr->bhqk', q_lat, lat)
              + jnp.einsum('bqhe,bke->bhqk', q_rope, kr)) * (C_NOPE + C_ROPE) ** -0.5
    p = masked_softmax(logits, chunk_causal(qpos, kpos))
    return jnp.einsum('bhqk,bkr->bqhr', p.astype(lat.dtype), lat)


def dsa_core(q, qi, wi, qpos, k, v, kidx, kpos, tab, topk):
    act = jax.nn.relu(jnp.einsum('bqhd,bkd->bqhk', qi, kidx) * IDX_DIM ** -0.5)
    score = jnp.einsum('bqh,bqhk->bqk', wi, act).astype(jnp.float32)
    score = jnp.where(chunk_causal(qpos, kpos), score, NEG_INF)
    _, sel = lax.top_k(score, topk)
    k_sel = jax.vmap(lambda kk, ii: kk[ii])(k, sel)
    v_sel = jax.vmap(lambda vv, ii: vv[ii])(v, sel)
    kpos_sel = kpos[sel]
    valid = (kpos_sel // CHUNK) <= (qpos // CHUNK)[None, :, None]
    bias = jnp.moveaxis(tab[t5_bucket(kpos_sel - qpos[None, :, None])], -1, 2)
    logits = jnp.einsum('bqhd,bqkhd->bqhk', q, k_sel) * DH_D ** -0.5 + bias
    p = masked_softmax(logits, valid[:, :, None, :])
    return jnp.einsum('bqhk,bqkhd->bqhd', p.astype(v.dtype), v_sel)


def token_mixers(h, l, past, w_in, a_rel_bias, t5_bias, b_lambda, b_subln,
                 c_q_norm, c_kv_norm, c_w_uq, c_w_uk, c_w_uv):
    bsz, T, _ = h.shape
    splits = np.cumsum(IN_SIZES)[:-1].tolist()
    (a_q, a_k, a_v, b_q, b_k, b_v, c_cq, c_ckv, c_kr,
     d_q, d_k, d_v, d_qi, d_ki, d_w) = jnp.split(h @ w_in[l], splits, axis=-1)
    past_len = 0 if past is None else past[1].shape[1]
    qpos = past_len + jnp.arange(T)
    kpos = jnp.arange(past_len + T)

    aq = a_q.reshape(bsz, T, H_A, DH_A)
    a_new = jnp.stack([a_k.reshape(bsz, T, H_A, DH_A), a_v.reshape(bsz, T, H_A, DH_A)], axis=2)
    if past is None:
        nc = T // CHUNK
        kvc = a_new.reshape(bsz, nc, CHUNK, 2, H_A, DH_A)
        kvc = jnp.pad(kvc, ((0, 0), (A_BAND_CHUNKS, 0), (0, 0), (0, 0), (0, 0), (0, 0)))
        band = jnp.concatenate([kvc[:, j:j + nc] for j in range(A_BAND_CHUNKS + 1)], axis=2)
        cid = jnp.arange(nc)[:, None] + jnp.arange(-A_BAND_CHUNKS, 1)[None, :]
        band_pos = (cid[:, :, None] * CHUNK + jnp.arange(CHUNK)).reshape(nc, -1)
        a_o = band_attention(aq.reshape(bsz, nc, CHUNK, H_A, DH_A), band[:, :, :, 0], band[:, :, :, 1],
                             qpos.reshape(nc, CHUNK), band_pos, a_rel_bias[l])
        a_state = a_new[:, T - min(A_WINDOW, T):]
    else:
        a_len = past[0].shape[1]
        kva = jnp.concatenate([past[0], a_new], axis=1)
        a_o = band_attention(aq[:, None], kva[:, None, :, 0], kva[:, None, :, 1], qpos[None],
                             jnp.arange(past_len - a_len, past_len + T)[None], a_rel_bias[l])
        a_state = a_new
    a_out = a_o.reshape(bsz, T, H_A * DH_A)

    bq = b_q.reshape(bsz, T, H_B, 2, DH_B)
    b_new = jnp.stack([b_k.reshape(bsz, T, H_B, 2 * DH_B), b_v.reshape(bsz, T, H_B, 2 * DH_B)], axis=2)
    kvb = b_new if past is None else jnp.concatenate([past[1], b_new], axis=1)
    bk = kvb[:, :, 0].reshape(bsz, past_len + T, H_B, 2, DH_B)
    bv = kvb[:, :, 1]
    lam_init = 0.8 - 0.6 * math.exp(-0.3 * l)
    lq1, lk1, lq2, lk2 = b_lambda[l].astype(jnp.float32)
    lam = jnp.exp(jnp.sum(lq1 * lk1)) - jnp.exp(jnp.sum(lq2 * lk2)) + lam_init
    tab_b = t5_bias[:, :H_B]
    b_o = over_query_blocks(lambda qs, qp: diff_core(qs[0], qp, bk, bv, kpos, tab_b, lam),
                            (bq,), qpos, Q_BLOCK)
    b_out = (rmsnorm(b_o, b_subln[l]) * (1.0 - lam_init)).reshape(bsz, T, H_B * 2 * DH_B)

    cqh = jnp.einsum('btr,rhe->bthe', rmsnorm(c_cq, c_q_norm[l]), c_w_uq[l])
    q_nope = cqh[..., :C_NOPE]
    q_rope = apply_rope(cqh[..., C_NOPE:], qpos)
    c_lat_new = rmsnorm(c_ckv, c_kv_norm[l])
    c_kr_new = apply_rope(c_kr, qpos)
    lat = c_lat_new if past is None else jnp.concatenate([past[2], c_lat_new], axis=1)
    kr = c_kr_new if past is None else jnp.concatenate([past[3], c_kr_new], axis=1)
    q_lat = jnp.einsum('bthn,rhn->bthr', q_nope, c_w_uk[l])
    o_lat = over_query_blocks(lambda qs, qp: mla_core(qs[0], qs[1], qp, lat, kr, kpos),
                              (q_lat, q_rope), qpos, Q_BLOCK)
    c_out = jnp.einsum('bthr,rhe->bthe', o_lat, c_w_uv[l]).reshape(bsz, T, H_C * C_V)

    dq = d_q.reshape(bsz, T, H_D, DH_D)
    d_new = jnp.stack([d_k.reshape(bsz, T, H_D, DH_D), d_v.reshape(bsz, T, H_D, DH_D)], axis=2)
    dqi = d_qi.reshape(bsz, T, IDX_HEADS, IDX_DIM)
    dw = d_w * IDX_HEADS ** -0.5
    kvd = d_new if past is None else jnp.concatenate([past[4], d_new], axis=1)
    kidx = d_ki if past is None else jnp.concatenate([past[5], d_ki], axis=1)
    dk_all = kvd[:, :, 0]
    dv_all = kvd[:, :, 1]
    topk = min(IDX_TOPK, (past_len + T) // 4)
    tab_d = t5_bias[:, H_B:]
    d_o = over_query_blocks(
        lambda qs, qp: dsa_core(qs[0], qs[1], qs[2], qp, dk_all, dv_all, kidx, kpos, tab_d, topk),
        (dq, dqi, dw), qpos, D_Q_BLOCK)
    d_out = d_o.reshape(bsz, T, H_D * DH_D)

    return (a_out, b_out, c_out, d_out), (a_state, b_new, c_lat_new, c_kr_new, d_new, d_ki)


def swiglu(h, w1, w2):
    g, u = jnp.split(h @ w1, 2, axis=-1)
    return (jax.nn.silu(g) * u) @ w2


def moe_swiglu(h, router, w1, w2):
    logits = (h @ router).astype(jnp.float32)
    top_v, top_i = lax.top_k(logits, TOP_K)
    wts = jax.nn.softmax(top_v, axis=-1)
    comb = jnp.sum(jax.nn.one_hot(top_i, N_EXPERTS, dtype=jnp.float32) * wts[..., None],
                   axis=-2).astype(h.dtype)
    y = None
    for e in range(N_EXPERTS):
        ye = comb[..., e:e + 1] * swiglu(h, w1[e], w2[e])
        y = ye if y is None else y + ye
    return y


def run_trunk(x, c, caches, w):
    (w_ada, b_ada, g_mix, g_ffn, g_final, w_in, a_rel_bias, t5_bias, b_lambda, b_subln,
     c_q_norm, c_kv_norm, c_w_uq, c_w_uk, c_w_uv, w_gate, w_branch, w_out,
     ffn_w1, ffn_w2, moe_router, moe_w1, moe_w2) = w
    states = []
    for l in range(DEPTH):
        past = None if caches is None else tuple(cc[l] for cc in caches)
        mod = jax.nn.silu(c) @ w_ada[l] + b_ada[l]
        sh1, sc1, g1, sh2, sc2, g2 = jnp.split(mod[:, None, :], 6, axis=-1)
        h = rmsnorm(x, g_mix[l]) * (1.0 + sc1) + sh1
        outs, st = token_mixers(h, l, past, w_in, a_rel_bias, t5_bias, b_lambda, b_subln,
                                c_q_norm, c_kv_norm, c_w_uq, c_w_uk, c_w_uv)
        merged = None
        for i in range(N_BRANCH):
            term = jax.nn.sigmoid(h @ w_gate[l, i]) * (outs[i] @ w_branch[l, i])
            merged = term if merged is None else merged + term
        x = x + g1 * (merged @ w_out[l])
        h = rmsnorm(x, g_ffn[l]) * (1.0 + sc2) + sh2
        if l % 2 == 0:
            f = swiglu(h, ffn_w1[l // 2], ffn_w2[l // 2])
        else:
            f = moe_swiglu(h, moe_router[l // 2], moe_w1[l // 2], moe_w2[l // 2])
        x = x + g2 * f
        states.append(st)
    y = rmsnorm(x, g_final)
    new_state = tuple(jnp.stack([s[i] for s in states], axis=0) for i in range(6))
    return y, new_state


def setup_inputs(seed: int = 0) -> dict:
    key = jax.random.key(seed)
    ks = iter(jax.random.split(key, 48))
    D = D_MODEL
    n_dense = (DEPTH + 1) // 2
    n_moe = DEPTH // 2
    a_len = min(A_WINDOW, PAST_LEN)

    def nrm(shape, scale):
        return jax.random.normal(next(ks), shape, jnp.float32) * scale

    def gain(shape):
        return jnp.ones(shape, jnp.float32) + nrm(shape, 0.01)

    return {
        'x_prompt': nrm((BATCH, SEQ, D), 1.0),
        'x_sample': nrm((DEC_BATCH, DEC_SEQ, D), 1.0),
        'c_prompt': nrm((BATCH, D), 1.0),
        'c_sample': nrm((DEC_BATCH, D), 1.0),
        'cache_a_kv': nrm((DEPTH, DEC_BATCH, a_len, 2, H_A, DH_A), 1.0),
        'cache_b_kv': nrm((DEPTH, DEC_BATCH, PAST_LEN, 2, H_B, 2 * DH_B), 1.0),
        'cache_c_latent': nrm((DEPTH, DEC_BATCH, PAST_LEN, C_KV_RANK), 1.0),
        'cache_c_krope': nrm((DEPTH, DEC_BATCH, PAST_LEN, C_ROPE), 1.0),
        'cache_d_kv': nrm((DEPTH, DEC_BATCH, PAST_LEN, 2, H_D, DH_D), 1.0),
        'cache_d_kidx': nrm((DEPTH, DEC_BATCH, PAST_LEN, IDX_DIM), 1.0),
        'w_ada': nrm((DEPTH, D, 6 * D), 0.5 * D ** -0.5),
        'b_ada': nrm((DEPTH, 6 * D), 0.02),
        'g_mix': gain((DEPTH, D)),
        'g_ffn': gain((DEPTH, D)),
        'g_final': gain((D,)),
        'w_in': nrm((DEPTH, D, IN_COLS), D ** -0.5),
        'a_rel_bias': nrm((DEPTH, 2 * A_CLIP + 1, H_A), 0.2),
        't5_bias': nrm((T5_BUCKETS, H_B + H_D), 0.2),
        'b_lambda': nrm((DEPTH, 4, DH_B), 0.1),
        'b_subln': gain((DEPTH, 2 * DH_B)),
        'c_q_norm': gain((DEPTH, C_Q_RANK)),
        'c_kv_norm': gain((DEPTH, C_KV_RANK)),
        'c_w_uq': nrm((DEPTH, C_Q_RANK, H_C, C_NOPE + C_ROPE), C_Q_RANK ** -0.5),
        'c_w_uk': nrm((DEPTH, C_KV_RANK, H_C, C_NOPE), C_KV_RANK ** -0.5),
        'c_w_uv': nrm((DEPTH, C_KV_RANK, H_C, C_V), C_KV_RANK ** -0.5),
        'w_gate': nrm((DEPTH, N_BRANCH, D, D), D ** -0.5),
        'w_branch': nrm((DEPTH, N_BRANCH, BRANCH_W, D), BRANCH_W ** -0.5),
        'w_out': nrm((DEPTH, D, D), D ** -0.5),
        'ffn_w1': nrm((n_dense, D, 2 * D_FF), D ** -0.5),
        'ffn_w2': nrm((n_dense, D_FF, D), D_FF ** -0.5),
        'moe_router': nrm((n_moe, D, N_EXPERTS), D ** -0.5),
        'moe_w1': nrm((n_moe, N_EXPERTS, D, 2 * D_FF_E), D ** -0.5),
        'moe_w2': nrm((n_moe, N_EXPERTS, D_FF_E, D), D_FF_E ** -0.5),
    }


def reference(x_prompt, x_sample, c_prompt, c_sample,
              cache_a_kv, cache_b_kv, cache_c_latent, cache_c_krope, cache_d_kv, cache_d_kidx,
              w_ada, b_ada, g_mix, g_ffn, g_final, w_in, a_rel_bias, t5_bias, b_lambda, b_subln,
              c_q_norm, c_kv_norm, c_w_uq, c_w_uk, c_w_uv, w_gate, w_branch, w_out,
              ffn_w1, ffn_w2, moe_router, moe_w1, moe_w2):
    weights = (w_ada, b_ada, g_mix, g_ffn, g_final, w_in, a_rel_bias, t5_bias, b_lambda, b_subln,
               c_q_norm, c_kv_norm, c_w_uq, c_w_uk, c_w_uv, w_gate, w_branch, w_out,
               ffn_w1, ffn_w2, moe_router, moe_w1, moe_w2)
    y_prompt, st_p = run_trunk(x_prompt, c_prompt, None, weights)
    caches = (cache_a_kv, cache_b_kv, cache_c_latent, cache_c_krope, cache_d_kv, cache_d_kidx)
    y_sample, st_s = run_trunk(x_sample, c_sample, caches, weights)
    return (y_prompt, y_sample,
            st_p[0], st_p[1], st_p[2], st_p[3], st_p[4], st_p[5],
            st_s[0], st_s[1], st_s[2], st_s[3], st_s[4], st_s[5])
```

```python
import contextlib
import math
import numpy as np
import concourse.bass as bass
import concourse.mybir as mybir
from concourse.bass_utils import run_bass_kernel_spmd

F32 = mybir.dt.float32
BF16 = mybir.dt.bfloat16
AF = mybir.ActivationFunctionType
ALU = mybir.AluOpType
AX = mybir.AxisListType

D = 2048
NKC = 16
IN_COLS = 5864
D_FF = 5632
D_FF_E = 7168
N_EXP = 8
EPS = 1e-6
NEG = -30000.0
PAST = 1024
A_PAST = 512
DSEQ = 64


class Op:
    __slots__ = ("eng", "fn", "deps", "is_dma", "signal", "ev", "pre")

    def __init__(self, eng, fn, is_dma):
        self.eng = eng
        self.fn = fn
        self.is_dma = is_dma
        self.deps = set()
        self.signal = False
        self.ev = None
        self.pre = None


class Prog:
    ENGS = ("pe", "act", "dve", "pool", "sp")
    SEM_ROLL = 30000
    NDMA_SEMS = 24

    def __init__(self, nc):
        self.nc = nc
        self.ops = {e: [] for e in self.ENGS}
        self.acc = {}
        self.pinfo = {}
        self.nops = 0
        self.bar_pos = {}

    def _box(self, ap):
        t = ap.tensor
        name = t.name
        off = ap.offset
        if str(ap.space) == "DRAM":
            span = 0
            for st, cnt in ap.ap:
                span += abs(st) * (cnt - 1)
            return name, (0, 0, off, off + span)
        P = self.pinfo.get(name)
        if P is None:
            P = 1
            for d in t.shape[1:]:
                P *= d
            self.pinfo[name] = P
        pspan = 0
        fspan = 0
        for st, cnt in ap.ap:
            if st >= P and st % P == 0:
                pspan += (st // P) * (cnt - 1)
            else:
                fspan += abs(st) * (cnt - 1)
        p0 = off // P
        f0 = off % P
        return name, (p0, p0 + pspan, f0, f0 + fspan)

    def _track(self, op, ap, is_write):
        name, box = self._box(ap)
        lst = self.acc.get(name)
        if lst is None:
            self.acc[name] = [(box, op, is_write)]
            return
        keep = []
        b0, b1, b2, b3 = box
        for rec in lst:
            rbox, rop, rw = rec
            if not (rbox[1] < b0 or b1 < rbox[0] or rbox[3] < b2 or b3 < rbox[2]):
                if (rw or is_write) and rop is not op:
                    same = (rop.eng == op.eng and not rop.is_dma and not op.is_dma)
                    if not (same and (is_write or op.eng == "pe")):
                        op.deps.add(rop)
                if is_write and b0 <= rbox[0] and b1 >= rbox[1] and b2 <= rbox[2] and b3 >= rbox[3]:
                    continue
                if (not is_write) and (not rw) and rop.eng == op.eng and not rop.is_dma and rbox == box:
                    continue
            keep.append(rec)
        keep.append((box, op, is_write))
        self.acc[name] = keep

    def op(self, eng, fn, reads=(), writes=(), dma=False):
        o = Op(eng, fn, dma)
        for ap in reads:
            self._track(o, ap, False)
        for ap in writes:
            self._track(o, ap, True)
        self.ops[eng].append(o)
        self.nops += 1
        return o

    def barrier(self):
        lasts = []
        dmas = []
        for e in self.ENGS:
            for o in reversed(self.ops[e]):
                if not o.is_dma and o.fn is not None:
                    lasts.append(o)
                    break
            for o in self.ops[e][self.bar_pos.get(e, 0):]:
                if o.is_dma:
                    dmas.append(o)
        for e in self.ENGS:
            self.bar_pos[e] = len(self.ops[e])
        for e in self.ENGS:
            o = Op(e, None, False)
            o.deps = set(x for x in lasts if x.eng != e) | set(dmas)
            self.ops[e].append(o)
        self.acc = {}

    def dma(self, out, in_, eng="sp", **kw):
        return self.op(eng, lambda e: e.dma_start(out=out, in_=in_, **kw), [in_], [out], dma=True)

    def mm(self, out, lhsT, rhs, start=True, stop=True, skip=False):
        return self.op("pe", lambda e: e.matmul(out, lhsT, rhs, start=start, stop=stop, skip_group_check=skip),
                       [lhsT, rhs], [out])

    def transpose(self, out, in_, ident):
        return self.op("pe", lambda e: e.transpose(out, in_, ident), [in_, ident], [out])

    def act(self, out, in_, func, bias=None, scale=None, accum_out=None):
        kw = {}
        rd = [in_]
        wr = [out]
        if bias is not None:
            kw["bias"] = bias
            if not isinstance(bias, (int, float)):
                rd.append(bias)
        if scale is not None:
            kw["scale"] = scale
            if not isinstance(scale, (int, float)):
                rd.append(scale)
        if accum_out is not None:
            kw["accum_out"] = accum_out
            wr.append(accum_out)
        return self.op("act", lambda e: e.activation(out, in_, func, **kw), rd, wr)

    def ts(self, out, in0, s1, s2, op0, op1=None, eng="dve"):
        rd = [in0]
        if s1 is not None and not isinstance(s1, (int, float)):
            rd.append(s1)
        if s2 is not None and not isinstance(s2, (int, float)):
            rd.append(s2)
        kw = {}
        if op1 is not None:
            kw["op1"] = op1
        return self.op(eng, lambda e: e.tensor_scalar(out, in0, s1, s2, op0, **kw), rd, [out])

    def tt(self, out, in0, in1, op, eng="dve"):
        return self.op(eng, lambda e: e.tensor_tensor(out, in0, in1, op), [in0, in1], [out])

    def stt(self, out, in0, scalar, in1, op0, op1):
        rd = [in0, in1]
        if not isinstance(scalar, (int, float)):
            rd.append(scalar)
        return self.op("dve", lambda e: e.scalar_tensor_tensor(out, in0, scalar, in1, op0, op1), rd, [out])

    def copy(self, out, in_, eng="dve"):
        if eng == "act":
            return self.op(eng, lambda e: e.copy(out, in_), [in_], [out])
        return self.op(eng, lambda e: e.tensor_copy(out, in_), [in_], [out])

    def memset(self, ap, val, eng="pool"):
        return self.op(eng, lambda e: e.memset(ap, val), [], [ap])

    def recip(self, out, in_):
        return self.op("dve", lambda e: e.reciprocal(out, in_), [in_], [out])

    def reduce(self, out, in_, op, axis=AX.X):
        return self.op("dve", lambda e: e.tensor_reduce(out, in_, axis, op), [in_], [out])

    def emit(self):
        nc = self.nc
        engs = self.ENGS
        for e in engs:
            for o in self.ops[e]:
                for d in o.deps:
                    d.signal = True
        nsem_needed = {}
        for e in engs:
            cnt = 0
            gen = 0
            for o in self.ops[e]:
                if o.is_dma or not o.signal:
                    continue
                if cnt >= self.SEM_ROLL:
                    gen += 1
                    cnt = 0
                cnt += 1
                o.ev = ("c", e, gen, cnt)
            nsem_needed[e] = gen + 1
        dma_count = {e: 0 for e in engs}
        for e in engs:
            for o in self.ops[e]:
                if not o.is_dma:
                    continue
                i = dma_count[e]
                dma_count[e] += 1
                k = i % self.NDMA_SEMS
                r = i // self.NDMA_SEMS
                o.ev = ("d", e, k, 16 * (r + 1))
                o.pre = ("d", e, k, 16 * r) if r > 0 else None
        with contextlib.ExitStack() as st:
            sems = {}
            for e in engs:
                for g in range(nsem_needed[e]):
                    sems[("c", e, g)] = st.enter_context(nc.semaphore(f"c_{e}_{g}"))
                for k in range(min(self.NDMA_SEMS, dma_count[e])):
                    sems[("d", e, k)] = st.enter_context(nc.semaphore(f"d_{e}_{k}"))
            block = st.enter_context(nc.Block())
            starters = {"pe": block.tensor, "act": block.scalar, "dve": block.vector,
                        "pool": block.gpsimd, "sp": block.sync}

            def make_body(e):
                ops = self.ops[e]

                def body(engobj):
                    waited = {}
                    for o in ops:
                        need = {}
                        for d in o.deps:
                            ev = d.ev
                            key = ev[:3]
                            if need.get(key, 0) < ev[3]:
                                need[key] = ev[3]
                        if o.pre is not None:
                            key = o.pre[:3]
                            if need.get(key, 0) < o.pre[3]:
                                need[key] = o.pre[3]
                        for key, val in need.items():
                            if waited.get(key, 0) >= val:
                                continue
                            waited[key] = val
                            engobj.wait_ge(sems[key], val)
                        if o.fn is None:
                            continue
                        ins = o.fn(engobj)
                        if o.is_dma:
                            ins.then_inc(sems[o.ev[:3]], 16)
                        elif o.signal:
                            ins.then_inc(sems[o.ev[:3]], 1)
                    last = {}
                    for o in ops:
                        if o.is_dma:
                            last[o.ev[:3]] = o.ev[3]
                    for key, val in last.items():
                        engobj.wait_ge(sems[key], val)
                return body

            for e in engs:
                if self.ops[e]:
                    starters[e](make_body(e))


def _t5_bucket_np(rel):
    nb = 16
    max_exact = 8
    ret = np.where(rel > 0, nb, 0)
    n = np.abs(rel)
    nf = np.maximum(n, 1).astype(np.float32)
    large = max_exact + (np.log(nf / np.float32(max_exact)) / np.float32(math.log(128 / max_exact))
                         * np.float32(nb - max_exact)).astype(np.int32)
    large = np.minimum(large, nb - 1)
    return ret + np.where(n < max_exact, n, large)


def make_consts(TP):
    c = {}
    c["ident"] = np.eye(128, dtype=np.float32)
    k = np.arange(128)[:, None]
    q = np.arange(128)[None, :]
    oh = np.zeros((2, 32, 128 * 128), np.float32)
    for d in range(2):
        b = _t5_bucket_np(k - q - 128 * d).reshape(-1)
        oh[d, b, np.arange(128 * 128)] = 1.0
    c["oh_t5"] = oh
    oha = np.zeros((3, 256, 128 * 128), np.float32)
    bases = [128, 0, 0]
    for d in range(3):
        idx = (np.clip(k - q - 128 * d, -256, 256) + 256 - bases[d]).reshape(-1)
        assert idx.min() >= 0 and idx.max() < 256
        oha[d, idx, np.arange(128 * 128)] = 1.0
    c["oh_a"] = oha
    m0 = np.zeros((128, 128), np.float32)
    m0[64:, :64] = NEG
    m4 = np.zeros((128, 128), np.float32)
    m4[:64, 64:] = NEG
    c["mask_t"] = np.stack([m0, m4])
    ms = np.zeros((128, 128), np.float32)
    ms[:64, 64:] = -1e30
    c["mask_s"] = ms
    half = 16
    freqs = (10000.0 ** (-np.arange(half, dtype=np.float32) / np.float32(half))).astype(np.float32)
    pos = np.concatenate([np.arange(TP), PAST + np.arange(DSEQ)]).astype(np.float32)
    ang = (pos[:, None] * freqs[None, :]).astype(np.float32)
    c["rope_cos"] = np.cos(ang).astype(np.float32)
    c["rope_sin"] = np.sin(ang).astype(np.float32)
    return c


class Cfg:
    def __init__(self, TP=4096, NS=8, depth=2, TS=512, phases="all", taps=()):
        self.TP = TP
        self.NS = NS
        self.depth = depth
        self.TS = TS
        self.NTOK = TP + NS * DSEQ
        self.NSEQ = 1 + NS
        self.KSEQ = PAST + DSEQ
        self.KSEQA = A_PAST + DSEQ
        self.NKEY = TP + NS * self.KSEQ
        self.NKEYA = TP + NS * self.KSEQA
        self.phases = phases
        self.taps = taps
        assert TP % TS == 0 and (NS * DSEQ) % 128 == 0 and TS % 128 == 0 and self.NTOK % TS == 0


WIN_BLOCKS = [
    ("a_q", 0, 512), ("a_k", 512, 512), ("a_v", 1024, 512),
    ("b_q", 1536, 512), ("b_k", 2048, 512), ("b_v", 2560, 512),
    ("c_cq", 3072, 384), ("c_kv", 3456, 288),
    ("d_q", 3744, 512), ("d_k", 4256, 512), ("d_v", 4768, 512),
    ("d_qi", 5280, 512), ("d_kw", 5792, 72),
]


class Builder:
    def __init__(self, cfg):
        self.cfg = cfg
        self.nc = bass.Bass("TRN2", target_bir_lowering=False)
        self.P = Prog(self.nc)
        self.uid = 0

    def din(self, name, shape, dt=F32):
        return self.nc.dram_tensor(name, list(shape), dt, kind="ExternalInput").ap()

    def dout(self, name, shape, dt=F32):
        return self.nc.dram_tensor(name, list(shape), dt, kind="ExternalOutput").ap()

    def dscr(self, name, shape, dt=F32):
        return self.nc.dram_tensor(name, list(shape), dt, kind="Internal").ap()

    def sb(self, name, shape, dt=F32):
        self.uid += 1
        return self.st.enter_context(self.nc.sbuf_tensor(f"{name}_{self.uid}", list(shape), dt))

    def sbp(self, name, shape, dt=F32):
        return self.root.enter_context(self.nc.sbuf_tensor(name, list(shape), dt))

    @contextlib.contextmanager
    def scope(self):
        old = self.st
        with contextlib.ExitStack() as inner:
            self.st = inner
            yield
            self.P.barrier()
        self.st = old

    def next_pm(self):
        self.pmi = (self.pmi + 1) % len(self.pm)
        return self.pm[self.pmi]

    def next_pt(self):
        self.pti = (self.pti + 1) % 2
        return self.ptb[self.pti]

    def evac(self, out, in_):
        self.evi += 1
        return self.P.copy(out, in_, eng=("act" if self.evi % 2 else "dve"))

    def tile_segs(self, t0):
        TP = self.cfg.TP
        if t0 < TP:
            return [(0, 128, 0)]
        s = (t0 - TP) // DSEQ
        return [(0, 64, 1 + s), (64, 128, 2 + s)]

    def key_base(self, seq, a=False):
        if seq == 0:
            return 0
        return self.cfg.TP + (seq - 1) * (self.cfg.KSEQA if a else self.cfg.KSEQ)

    def tok_keyrows(self, t0, a=False):
        TP = self.cfg.TP
        if t0 < TP:
            return [(0, 128, t0)]
        s = (t0 - TP) // DSEQ
        past = A_PAST if a else PAST
        return [(0, 64, self.key_base(1 + s, a) + past), (64, 128, self.key_base(2 + s, a) + past)]

    def declare(self):
        c = self.cfg
        L, NS, NTOK, NSEQ, TP = c.depth, c.NS, c.NTOK, c.NSEQ, c.TP
        NKEY, NKEYA = c.NKEY, c.NKEYA
        d = self.din
        self.x_all = d("x_all", [NTOK, D])
        self.c_all = d("c_all", [NSEQ, D])
        self.ca_kv = d("ca_kv", [L, NS, A_PAST, 1024])
        self.cb_kv = d("cb_kv", [L, NS, PAST, 1024])
        self.cc_lat = d("cc_lat", [L, NS, PAST, 256])
        self.cc_kr = d("cc_kr", [L, NS, PAST, 32])
        self.cd_kv = d("cd_kv", [L, NS, PAST, 1024])
        self.cd_ki = d("cd_ki", [L, NS, PAST, 64])
        self.w_ada = d("w_ada", [L, D, 6 * D])
        self.b_ada = d("b_ada", [L, 6 * D])
        self.g_mix = d("g_mix", [L, D])
        self.g_ffn = d("g_ffn", [L, D])
        self.g_final = d("g_final", [1, D])
        self.w_in = d("w_in", [L, D, IN_COLS])
        self.a_rel = d("a_rel", [L, 513, 8])
        self.t5_bias = d("t5_bias", [32, 12])
        self.b_lambda = d("b_lambda", [L, 256])
        self.b_subln = d("b_subln", [L, 128])
        self.c_q_norm = d("c_q_norm", [L, 384])
        self.c_kv_norm = d("c_kv_norm", [L, 256])
        self.c_w_uq = d("c_w_uq", [L, 384, 384])
        self.c_w_uk = d("c_w_uk", [L, 256, 256])
        self.c_w_uv = d("c_w_uv", [L, 256, 512])
        self.w_gate = d("w_gate", [L, 4, D, D])
        self.w_branch = d("w_branch", [L, 4, 512, D])
        self.w_out = d("w_out", [L, D, D])
        self.ffn_w1 = d("ffn_w1", [1, D, 2 * D_FF])
        self.ffn_w2 = d("ffn_w2", [1, D_FF, D])
        if L > 1:
            self.moe_router = d("moe_router", [1, D, N_EXP])
            self.moe_w1 = d("moe_w1", [1, N_EXP, D, 2 * D_FF_E])
            self.moe_w2 = d("moe_w2", [1, N_EXP, D_FF_E, D])
        self.ident_in = d("ident", [128, 128])
        self.oh_t5 = d("oh_t5", [2, 32, 16384])
        self.oh_a = d("oh_a", [3, 256, 16384])
        self.mask_t = d("mask_t", [2, 128, 128])
        self.mask_s = d("mask_s", [128, 128])
        self.rope_cos = d("rope_cos", [TP + DSEQ, 16])
        self.rope_sin = d("rope_sin", [TP + DSEQ, 16])
        o = self.dout
        self.y_all = o("y_all", [NTOK, D])
        self.st_a = o("st_a", [L, NTOK, 1024])
        self.st_b = o("st_b", [L, NTOK, 1024])
        self.st_cl = o("st_cl", [L, NTOK, 256])
        self.st_ck = o("st_ck", [L, NTOK, 32])
        self.st_d = o("st_d", [L, NTOK, 1024])
        self.st_di = o("st_di", [L, NTOK, 64])
        s = self.dscr
        self.xres = s("xres", [NTOK, D])
        self.modv = s("modv", [L, 6, NSEQ, D])
        self.qT_a = s("qT_a", [512, NTOK], BF16)
        self.kT_a = s("kT_a", [512, NKEYA], BF16)
        self.v_a = s("v_a", [NKEYA, 512], BF16)
        self.qT_b = s("qT_b", [512, NTOK], BF16)
        self.kT_b = s("kT_b", [512, NKEY], BF16)
        self.v_b = s("v_b", [NKEY, 512], BF16)
        self.qT_c = s("qT_c", [4, 288, NTOK], BF16)
        self.latT = s("latT", [256, NKEY], BF16)
        self.krT = s("krT", [32, NKEY], BF16)
        self.lat_v = s("lat_v", [NKEY, 256], BF16)
        self.qT_d = s("qT_d", [512, NTOK], BF16)
        self.kT_d = s("kT_d", [512, NKEY], BF16)
        self.v_d = s("v_d", [NKEY, 512], BF16)
        self.qiT = s("qiT", [512, NTOK], BF16)
        self.kiT = s("kiT", [64, NKEY], BF16)
        self.dw_s = s("dw_s", [NTOK, 8])
        self.OT = s("OT", [2048, NTOK], BF16)
        self.bt_scr = s("bt_scr", [12, 2, 16384])
        self.bta_scr = s("bta_scr", [L, 8, 3, 16384])
        self.tap_out = {}
        for name in c.taps:
            src = getattr(self, name)
            self.tap_out[name] = o("tap_" + name, list(src.shape), src.dtype)

    def common_tiles(self):
        c = self.cfg
        P, sb = self.P, self.sbp
        TS = c.TS
        self.pm = [self.st.enter_context(self.nc.psum_tensor(f"pm{i}", [128, 512], F32)) for i in range(6)]
        self.ptb = [self.st.enter_context(self.nc.psum_tensor(f"ptb{i}", [128, 1024], BF16)) for i in range(2)]
        self.pmi = 0
        self.pti = 0
        self.evi = 0
        self.identf = sb("identf", [128, 128])
        self.identb = sb("identb", [128, 128], BF16)
        self.onesb = sb("onesb", [128, 128], BF16)
        self.onesf = sb("onesf", [128, 128])
        P.dma(self.identf[:], self.ident_in)
        P.copy(self.identb[:], self.identf[:])
        P.memset(self.onesb[:], 1.0)
        P.memset(self.onesf[:], 1.0)
        self.NT_S = TS // 128
        self.ss = sb("ss", [128, 8])
        self.wai = 0

    def scoped_common(self, need_tmpf=True):
        self.wA = [self.sb(f"wA{i}", [128, NKC, 512], BF16) for i in range(2)]
        if need_tmpf:
            self.tmpf = self.sb("tmpf", [128, D])

    def next_wA(self):
        self.wai = (self.wai + 1) % 2
        return self.wA[self.wai]

    def rms_rstd(self, src, width, col):
        P, ss = self.P, self.ss
        P.act(self.junk[:, 0:width], src, AF.Square, accum_out=ss[:, col:col + 1])
        P.ts(ss[:, col:col + 1], ss[:, col:col + 1], 1.0 / width, EPS, ALU.mult, ALU.add)
        P.act(ss[:, col:col + 1], ss[:, col:col + 1], AF.Sqrt)
        P.recip(ss[:, col:col + 1], ss[:, col:col + 1])

    def load_bc(self, dst, l, slot, t0):
        for (r0, r1, seq) in self.tile_segs(t0):
            self.P.dma(dst[r0:r1, :], self.modv[l, slot, seq:seq + 1, :].broadcast_to([r1 - r0, D]))

    def load_w(self, dst, src2d, rows, cols, eng="pool"):
        kc = rows // 128
        self.P.dma(dst[:, 0:kc, 0:cols], src2d.rearrange("(k p) c -> p k c", p=128), eng=eng)

    def norm_mod_to_hT(self, l, slotA, slotS, t0, j, xtile):
        P = self.P
        self.load_bc(self.bcA, l, slotA, t0)
        self.load_bc(self.bcB, l, slotS, t0)
        self.rms_rstd(xtile, D, 0)
        P.stt(self.tmpf[:], xtile, self.ss[:, 0:1], self.bcA[:], ALU.mult, ALU.mult)
        P.tt(self.hb[:], self.tmpf[:], self.bcB[:], ALU.add)
        self.to_hT(self.hb, j)

    def to_hT(self, src_bf16, j):
        P = self.P
        for half in range(2):
            pt = self.next_pt()
            for k in range(8):
                kc = half * 8 + k
                P.transpose(pt[:, k * 128:(k + 1) * 128], src_bf16[:, kc * 128:(kc + 1) * 128], self.identb[:])
            self.evac(self.hT[:, half * 8:(half + 1) * 8, j * 128:(j + 1) * 128],
                      pt[:].rearrange("p (k t) -> p k t", k=8))

    def phase0(self):
        c = self.cfg
        P, sb = self.P, self.sb
        L, NSEQ = c.depth, c.NSEQ
        csb = sb("csb", [NSEQ, D])
        csbb = sb("csbb", [NSEQ, D], BF16)
        cT = sb("cT", [128, NKC, NSEQ], BF16)
        modrow = sb("modrow", [NSEQ, 6 * D])
        grow = sb("grow", [NSEQ, D])
        P.dma(csb[:], self.c_all)
        P.act(csbb[:], csb[:], AF.Silu)
        pt = self.next_pt()
        for kc in range(NKC):
            P.transpose(pt[:, kc * 16:kc * 16 + NSEQ], csbb[:, kc * 128:(kc + 1) * 128],
                        self.identb[0:NSEQ, 0:NSEQ])
        P.copy(cT[:], pt[:, 0:NKC * 16].rearrange("p (k t) -> p k t", k=NKC)[:, :, 0:NSEQ])
        for l in range(L):
            P.dma(modrow[:], self.b_ada[l:l + 1, :].broadcast_to([NSEQ, 6 * D]))
            for cb in range(24):
                w = self.next_wA()
                self.load_w(w, self.w_ada[l, :, cb * 512:(cb + 1) * 512], D, 512)
                pp = self.next_pm()
                for kc in range(NKC):
                    P.mm(pp[0:NSEQ, :], cT[:, kc, :], w[:, kc, :], start=(kc == 0), stop=(kc == NKC - 1))
                P.tt(modrow[:, cb * 512:(cb + 1) * 512], modrow[:, cb * 512:(cb + 1) * 512], pp[0:NSEQ, :], ALU.add)
            for (gsrc, sc_i, sh_i, g_i, base) in ((self.g_mix, 1, 0, 2, 0), (self.g_ffn, 4, 3, 5, 3)):
                P.dma(grow[:], gsrc[l:l + 1, :].broadcast_to([NSEQ, D]))
                P.stt(grow[:], modrow[:, sc_i * D:(sc_i + 1) * D], 1.0, grow[:], ALU.add, ALU.mult)
                P.dma(self.modv[l, base + 0], grow[:])
                P.dma(self.modv[l, base + 1], modrow[:, sh_i * D:(sh_i + 1) * D])
                P.dma(self.modv[l, base + 2], modrow[:, g_i * D:(g_i + 1) * D])

    def bias_alloc(self):
        L = self.cfg.depth
        self.mt = self.sbp("mt", [128, 2, 128])
        self.msk_s = self.sbp("msk_s", [128, 128])
        self.cb_t5 = self.sbp("cb_t5", [128, 12])
        self.cb_a = self.sbp("cb_a", [128, L, 8])

    def bias_tiles(self):
        c = self.cfg
        P, sb = self.P, self.sb
        L = c.depth
        tabsb = sb("tabsb", [32, 12])
        ohsb = sb("ohsb", [128, 2, 512])
        rowsb = sb("rowsb", [12, 512])
        P.dma(self.mt[:], self.mask_t.rearrange("m k q -> k m q"))
        P.dma(self.msk_s[:], self.mask_s)
        P.dma(self.cb_t5[:], self.t5_bias[15:16, :].broadcast_to([128, 12]))
        P.dma(tabsb[:], self.t5_bias)
        for d in range(2):
            for ch in range(32):
                P.dma(ohsb[0:32, 0, :], self.oh_t5[d, :, ch * 512:(ch + 1) * 512])
                pp = self.next_pm()
                P.mm(pp[0:12, :], tabsb[:, :], ohsb[0:32, 0, :])
                self.evac(rowsb[:, :], pp[0:12, :])
                P.dma(self.bt_scr[:, d, ch * 512:(ch + 1) * 512], rowsb[:, :])
        atab = sb("atab", [128, 3, 8])
        for l in range(L):
            P.dma(self.cb_a[:, l, :], self.a_rel[l, 0:1, :].broadcast_to([128, 8]))
            P.dma(atab[:, :, :], self.a_rel[l, 0:384, :].rearrange("(c p) h -> p c h", p=128))
            bases = [128, 0, 0]
            for d in range(3):
                c0 = bases[d] // 128
                for ch in range(32):
                    P.dma(ohsb[:, :, :], self.oh_a[d, :, ch * 512:(ch + 1) * 512].rearrange("(c p) n -> p c n", p=128))
                    pp = self.next_pm()
                    for cc in range(2):
                        P.mm(pp[0:8, :], atab[:, c0 + cc, :], ohsb[:, cc, :], start=(cc == 0), stop=(cc == 1))
                    self.evac(rowsb[0:8, :], pp[0:8, :])
                    P.dma(self.bta_scr[l, :, d, ch * 512:(ch + 1) * 512], rowsb[0:8, :])

    def load_bias_tiles(self, l):
        P = self.P
        self.BT = self.sb("BT", [128, 12, 2, 128])
        self.BTA = self.sb("BTA", [128, 8, 4, 128])
        for h in range(12):
            for d in range(2):
                P.dma(self.BT[:, h, d, :], self.bt_scr[h, d, :].rearrange("(k q) -> k q", q=128))
            P.tt(self.BT[:, h, 0, :], self.BT[:, h, 0, :], self.mt[:, 0, :], ALU.add)
        for h in range(8):
            for d in range(3):
                P.dma(self.BTA[:, h, d, :], self.bta_scr[l, h, d, :].rearrange("(k q) -> k q", q=128))
            P.tt(self.BTA[:, h, 0, :], self.BTA[:, h, 0, :], self.mt[:, 0, :], ALU.add)
            P.ts(self.BTA[:, h, 3, :], self.mt[:, 1, :], self.cb_a[:, l, h:h + 1], None, ALU.add)

    def param_tiles(self):
        sb = self.sbp
        self.cqn_bc = sb("cqn_bc", [128, 384])
        self.ckvn_bc = sb("ckvn_bc", [128, 256])
        self.wuq = sb("wuq", [128, 3, 384], BF16)
        self.wukf = sb("wukf", [128, 2, 256])
        self.wukT = sb("wukT", [128, 4, 256], BF16)
        self.wuv = sb("wuv", [128, 2, 512], BF16)
        self.lam_t = sb("lam_t", [128, 8])
        self.lamw = sb("lamw", [128, 256])
        self.subg = sb("subg", [128, 1])
        self.cos_t = sb("cos_t", [128, 16])
        self.sin_t = sb("sin_t", [128, 16])

    def layer_params(self, l):
        P = self.P
        P.dma(self.cqn_bc[:], self.c_q_norm[l:l + 1, :].broadcast_to([128, 384]))
        P.dma(self.ckvn_bc[:], self.c_kv_norm[l:l + 1, :].broadcast_to([128, 256]))
        P.dma(self.wuq[:], self.c_w_uq[l].rearrange("(k p) c -> p k c", p=128), eng="pool")
        P.dma(self.wuv[:], self.c_w_uv[l].rearrange("(k p) c -> p k c", p=128), eng="pool")
        P.dma(self.wukf[:], self.c_w_uk[l].rearrange("(k p) c -> p k c", p=128))
        for hp in range(2):
            pp = self.next_pm()
            for cc in range(2):
                P.transpose(pp[:, cc * 128:(cc + 1) * 128], self.wukf[:, cc, hp * 128:(hp + 1) * 128], self.identf[:])
            self.evac(self.wukT[0:64, 2 * hp, :], pp[0:64, 0:256])
            self.evac(self.wukT[64:128, 2 * hp + 1, :], pp[64:128, 0:256])
        lam_init = 0.8 - 0.6 * math.exp(-0.3 * l)
        lamw, lam_t = self.lamw, self.lam_t
        P.dma(lamw[:], self.b_lambda[l:l + 1, :].broadcast_to([128, 256]))
        P.tt(lamw[:, 0:64], lamw[:, 0:64], lamw[:, 64:128], ALU.mult)
        P.tt(lamw[:, 128:192], lamw[:, 128:192], lamw[:, 192:256], ALU.mult)
        P.reduce(lam_t[:, 0:1], lamw[:, 0:64], ALU.add)
        P.reduce(lam_t[:, 1:2], lamw[:, 128:192], ALU.add)
        P.act(lam_t[:, 2:4], lam_t[:, 0:2], AF.Exp)
        P.tt(lam_t[:, 4:5], lam_t[:, 3:4], lam_t[:, 2:3], ALU.subtract)
        P.ts(lam_t[:, 5:6], lam_t[:, 4:5], -lam_init, None, ALU.add)
        P.dma(self.subg[:], self.b_subln[l:l + 1, :].rearrange("o e -> e o"))
        P.ts(self.subg[:], self.subg[:], 1.0 - lam_init, None, ALU.mult)

    def p1_tiles(self):
        c = self.cfg
        sb = self.sb
        TS = c.TS
        self.xt = sb("xt", [128, self.NT_S, D])
        self.hT = sb("hT", [128, NKC, TS], BF16)
        self.hb = sb("hb", [128, D], BF16)
        self.bcA = sb("bcA", [128, D])
        self.bcB = sb("bcB", [128, D])
        self.stg = sb("stg", [128, 512])
        self.stgb = sb("stgb", [128, 512], BF16)
        self.ftile = sb("ftile", [128, 4, TS], BF16)
        self.cqf = sb("cqf", [128, 384])
        self.cqb = sb("cqb", [128, 384], BF16)
        self.cqT = sb("cqT", [128, 3, 128], BF16)
        self.cqh = sb("cqh", [128, 384])
        self.qnb = sb("qnb", [128, 256], BF16)
        self.qrf = sb("qrf", [128, 4, 32])
        self.qrb = sb("qrb", [128, 128], BF16)
        self.qnT = sb("qnT", [128, 2, 128], BF16)
        self.qcs = sb("qcs", [128, 2, 128], BF16)
        self.rt1 = sb("rt1", [128, 4, 16])
        self.rt2 = sb("rt2", [128, 4, 16])
        self.kvf = sb("kvf", [128, 288])
        self.latf = sb("latf", [128, 256])
        self.latb = sb("latb", [128, 256], BF16)
        self.krf = sb("krf", [128, 32])
        self.krb = sb("krb", [128, 32], BF16)
        self.smallT = sb("smallT", [128, 4, 128], BF16)
        self.kwf = sb("kwf", [128, 72])
        self.kib = sb("kib", [128, 64], BF16)

    def load_rope(self, t0):
        P, TP = self.P, self.cfg.TP
        if t0 < TP:
            P.dma(self.cos_t[:], self.rope_cos[t0:t0 + 128, :])
            P.dma(self.sin_t[:], self.rope_sin[t0:t0 + 128, :])
        else:
            for r0 in (0, 64):
                P.dma(self.cos_t[r0:r0 + 64, :], self.rope_cos[TP:TP + 64, :])
                P.dma(self.sin_t[r0:r0 + 64, :], self.rope_sin[TP:TP + 64, :])

    def rope_tm(self, d1, d2, x1, x2, nh):
        P = self.P
        if nh > 1:
            cosb = self.cos_t[:].unsqueeze(1).to_broadcast([128, nh, 16])
            sinb = self.sin_t[:].unsqueeze(1).to_broadcast([128, nh, 16])
            t1 = self.rt1[:, 0:nh, :]
            t2 = self.rt2[:, 0:nh, :]
        else:
            cosb = self.cos_t[:]
            sinb = self.sin_t[:]
            t1 = self.rt1[:, 0, :]
            t2 = self.rt2[:, 0, :]
        P.tt(t1, x1, cosb, ALU.mult)
        P.tt(t2, x2, sinb, ALU.mult)
        P.tt(d1, t1, t2, ALU.subtract)
        P.tt(t1, x2, cosb, ALU.mult)
        P.tt(t2, x1, sinb, ALU.mult)
        P.tt(d2, t1, t2, ALU.add)

    def phase1(self, l):
        c = self.cfg
        P = self.P
        TS, NTOK, TP = c.TS, c.NTOK, c.TP
        NT_S = self.NT_S
        xsrc = self.x_all if l == 0 else self.xres
        hT = self.hT
        for s0 in range(0, NTOK, TS):
            for j in range(NT_S):
                t0 = s0 + j * 128
                P.dma(self.xt[:, j, :], xsrc[t0:t0 + 128, :])
                self.norm_mod_to_hT(l, 0, 1, t0, j, self.xt[:, j, :])
            for (name, c0, wd) in WIN_BLOCKS:
                w = self.next_wA()
                self.load_w(w, self.w_in[l, :, c0:c0 + wd], D, wd)
                feat = name in ("a_q", "a_k", "b_q", "b_k", "d_q", "d_k", "d_qi")
                tokm = name not in ("a_q", "b_q", "d_q", "d_qi")
                if feat:
                    for cc in range(4):
                        pp = self.next_pm()
                        for kc in range(NKC):
                            P.mm(pp[:, 0:TS], w[:, kc, cc * 128:(cc + 1) * 128], hT[:, kc, :],
                                 start=(kc == 0), stop=(kc == NKC - 1))
                        self.evac(self.ftile[:, cc, :], pp[:, 0:TS])
                    if name[2] == "q":
                        dst = {"a_q": self.qT_a, "b_q": self.qT_b, "d_q": self.qT_d, "d_qi": self.qiT}[name]
                        P.dma(dst[:, s0:s0 + TS].rearrange("(c p) t -> p c t", p=128), self.ftile[:])
                    else:
                        isa = (name == "a_k")
                        dst = {"a_k": self.kT_a, "b_k": self.kT_b, "d_k": self.kT_d}[name]
                        for j in range(NT_S):
                            for (r0, r1, k0) in self.tok_keyrows(s0 + j * 128, isa):
                                P.dma(dst[:, k0:k0 + (r1 - r0)].rearrange("(c p) t -> p c t", p=128),
                                      self.ftile[:, :, j * 128 + r0:j * 128 + r1])
                if not tokm:
                    continue
                for j in range(NT_S):
                    t0 = s0 + j * 128
                    pp = self.next_pm()
                    for kc in range(NKC):
                        P.mm(pp[:, 0:wd], hT[:, kc, j * 128:(j + 1) * 128], w[:, kc, 0:wd],
                             start=(kc == 0), stop=(kc == NKC - 1))
                    if name in ("a_k", "b_k", "d_k", "a_v", "b_v", "d_v"):
                        sto = {"a": self.st_a, "b": self.st_b, "d": self.st_d}[name[0]]
                        isv = name[2] == "v"
                        self.evac(self.stg[:], pp[:])
                        P.dma(sto[l, t0:t0 + 128, (512 if isv else 0):(1024 if isv else 512)], self.stg[:])
                        if isv:
                            P.copy(self.stgb[:], self.stg[:], eng="pool")
                            vdst = {"a": self.v_a, "b": self.v_b, "d": self.v_d}[name[0]]
                            for (r0, r1, k0) in self.tok_keyrows(t0, name[0] == "a"):
                                P.dma(vdst[k0:k0 + (r1 - r0), :], self.stgb[r0:r1, :])
                    elif name == "c_cq":
                        self.p1_cq(l, t0, pp)
                    elif name == "c_kv":
                        self.p1_ckv(l, t0, pp)
                    elif name == "d_kw":
                        self.p1_dkw(l, t0, pp)

    def p1_cq(self, l, t0, pp):
        P = self.P
        self.evac(self.cqf[:], pp[:, 0:384])
        self.rms_rstd(self.cqf[:], 384, 1)
        P.stt(self.cqb[:], self.cqf[:], self.ss[:, 1:2], self.cqn_bc[:], ALU.mult, ALU.mult)
        pt = self.next_pt()
        for cc in range(3):
            P.transpose(pt[:, cc * 128:(cc + 1) * 128], self.cqb[:, cc * 128:(cc + 1) * 128], self.identb[:])
        self.evac(self.cqT[:], pt[:, 0:384].rearrange("p (k t) -> p k t", k=3))
        p2 = self.next_pm()
        for cc in range(3):
            P.mm(p2[:, 0:384], self.cqT[:, cc, :], self.wuq[:, cc, :], start=(cc == 0), stop=(cc == 2))
        self.evac(self.cqh[:], p2[:, 0:384])
        cq4 = self.cqh[:].rearrange("p (h e) -> p h e", h=4)
        P.copy(self.qnb[:].rearrange("p (h n) -> p h n", h=4), cq4[:, :, 0:64], eng="pool")
        self.load_rope(t0)
        self.rope_tm(self.qrf[:, :, 0:16], self.qrf[:, :, 16:32], cq4[:, :, 64:80], cq4[:, :, 80:96], 4)
        P.copy(self.qrb[:], self.qrf[:].rearrange("p h e -> p (h e)"), eng="pool")
        pt = self.next_pt()
        for cc in range(2):
            P.transpose(pt[:, cc * 128:(cc + 1) * 128], self.qnb[:, cc * 128:(cc + 1) * 128], self.identb[:])
        P.transpose(pt[:, 256:384], self.qrb[:], self.identb[:])
        self.evac(self.qnT[:], pt[:, 0:256].rearrange("p (k t) -> p k t", k=2))
        self.evac(self.smallT[:, 0, :], pt[:, 256:384])
        for h in range(4):
            po = (h % 2) * 64
            cc = h // 2
            p3 = self.next_pm()
            for rc in range(2):
                P.mm(p3[:, rc * 128:(rc + 1) * 128], self.wukT[po:po + 64, h, rc * 128:(rc + 1) * 128],
                     self.qnT[po:po + 64, cc, :], start=True, stop=True)
            self.evac(self.qcs[:], p3[:, 0:256].rearrange("p (k t) -> p k t", k=2))
            P.dma(self.qT_c[h, 0:256, t0:t0 + 128].rearrange("(c p) t -> p c t", p=128), self.qcs[:])
            P.dma(self.qT_c[h, 256:288, t0:t0 + 128], self.smallT[h * 32:(h + 1) * 32, 0, :])

    def p1_ckv(self, l, t0, pp):
        P = self.P
        kvf = self.kvf
        self.evac(kvf[:], pp[:, 0:288])
        self.rms_rstd(kvf[:, 0:256], 256, 2)
        P.stt(self.latf[:], kvf[:, 0:256], self.ss[:, 2:3], self.ckvn_bc[:], ALU.mult, ALU.mult)
        P.dma(self.st_cl[l, t0:t0 + 128, :], self.latf[:])
        P.copy(self.latb[:], self.latf[:], eng="pool")
        self.load_rope(t0)
        self.rope_tm(self.krf[:, 0:16], self.krf[:, 16:32], kvf[:, 256:272], kvf[:, 272:288], 1)
        P.dma(self.st_ck[l, t0:t0 + 128, :], self.krf[:])
        P.copy(self.krb[:], self.krf[:], eng="pool")
        pt = self.next_pt()
        for cc in range(2):
            P.transpose(pt[:, cc * 128:(cc + 1) * 128], self.latb[:, cc * 128:(cc + 1) * 128], self.identb[:])
        P.transpose(pt[0:32, 256:384], self.krb[:], self.identb[:])
        self.evac(self.smallT[:, 1:3, :], pt[:, 0:256].rearrange("p (k t) -> p k t", k=2))
        self.evac(self.smallT[0:32, 3, :], pt[0:32, 256:384])
        for (r0, r1, k0) in self.tok_keyrows(t0):
            n = r1 - r0
            P.dma(self.lat_v[k0:k0 + n, :], self.latb[r0:r1, :])
            P.dma(self.latT[:, k0:k0 + n].rearrange("(c p) t -> p c t", p=128), self.smallT[:, 1:3, r0:r1])
            P.dma(self.krT[:, k0:k0 + n], self.smallT[0:32, 3, r0:r1])

    def p1_dkw(self, l, t0, pp):
        P = self.P
        kwf = self.kwf
        self.evac(kwf[:], pp[:, 0:72])
        P.dma(self.st_di[l, t0:t0 + 128, :], kwf[:, 0:64])
        P.copy(self.kib[:], kwf[:, 0:64], eng="pool")
        P.ts(kwf[:, 64:72], kwf[:, 64:72], 8.0 ** -0.5, None, ALU.mult)
        P.dma(self.dw_s[t0:t0 + 128, :], kwf[:, 64:72])
        pt = self.next_pt()
        P.transpose(pt[0:64, 0:128], self.kib[:], self.identb[:])
        self.evac(self.smallT[0:64, 0, :], pt[0:64, 0:128])
        for (r0, r1, k0) in self.tok_keyrows(t0):
            P.dma(self.kiT[:, k0:k0 + (r1 - r0)], self.smallT[0:64, 0, r0:r1])

    def emit_taps(self):
        for name, dst in self.tap_out.items():
            src = getattr(self, name)
            self.P.dma(dst, src)

    def build(self):
        c = self.cfg
        with contextlib.ExitStack() as st:
            self.st = st
            self.root = st
            self.declare()
            self.common_tiles()
            self.bias_alloc()
            self.param_tiles()
            ph = c.phases
            with self.scope():
                self.scoped_common(False)
                self.phase0()
            with self.scope():
                self.bias_tiles()
            for l in range(c.depth):
                self.layer_params(l)
                with self.scope():
                    self.scoped_common()
                    self.p1_tiles()
                    self.junk = self.hb
                    self.phase1(l)
                if ph in ("p1",):
                    continue
                with self.scope():
                    self.p2_tiles()
                    self.load_bias_tiles(l)
                    self.cache_prep(l)
                    self.phase2(l)
                if ph in ("p2",):
                    continue
                with self.scope():
                    self.scoped_common()
                    self.p3_tiles()
                    self.junk = self.hb
                    self.phase3(l)
            self.P.barrier()
            self.emit_taps()
            self.P.emit()
        return self.nc


def _p2_tiles(self):
    c = self.cfg
    sb = self.sb
    NKMAX = max(c.TP, c.KSEQ)
    NKB = (NKMAX + 127) // 128
    self.NKMAX, self.NKB = NKMAX, NKB
    self.kTs = sb("kTs", [128, 4, NKMAX], BF16)
    self.vs = sb("vs", [128, NKB, 512], BF16)
    self.kiT2 = sb("kiT2", [128, NKMAX], BF16)
    self.qs = sb("qs", [128, 4, 128], BF16)
    self.qs_c = sb("qs_c", [128, 4, 3, 128], BF16)
    self.qis = sb("qis", [128, 4, 128], BF16)
    self.dws = sb("dws", [128, 8])
    self.pTs = [sb(f"pT{i}", [128, 128], BF16) for i in range(4)]
    self.pTi = 0
    self.ntmp = [sb(f"ntmp{i}", [128, 128]) for i in range(2)]
    self.nti = 0
    self.sc = sb("sc", [128, NKMAX])
    self.wk = sb("wk", [128, NKMAX])
    self.itmp = [sb(f"itmp{i}", [128, 512]) for i in range(2)]
    self.iti = 0
    self.m8 = sb("m8", [128, 8])
    self.sel = sb("sel", [128, NKMAX], BF16)
    self.selT = sb("selT", [128, NKB, 128], BF16)
    self.fz = [sb(f"fz{i}", [128, 128]) for i in range(6)]
    self.olat = sb("olat", [128, 2, 128], BF16)
    self.ot = [sb(f"ot{i}", [128, 128], BF16) for i in range(2)]
    self.oti = 0
    self.ota = sb("ota", [64, 8, 128], BF16)
    self.cpl = sb("cpl", [128, 1024], BF16)
    self.cpT = sb("cpT", [128, 4, 128], BF16)
    self.s_slot = 0
    self.accset = 0


def _cache_prep(self, l):
    c = self.cfg
    P = self.P
    for s in range(c.NS):
        seq = 1 + s
        for (src, past, wd, a) in ((self.ca_kv, A_PAST, 1024, True), (self.cb_kv, PAST, 1024, False),
                                   (self.cd_kv, PAST, 1024, False), (self.cc_lat, PAST, 256, False),
                                   (self.cc_kr, PAST, 32, False), (self.cd_ki, PAST, 64, False)):
            kb = self.key_base(seq, a)
            for i in range(past // 128):
                P.dma(self.cpl[:, 0:wd], src[l, s, i * 128:(i + 1) * 128, :], eng="pool")
                k0 = kb + i * 128
                if wd == 1024:
                    kT, vv = {id(self.ca_kv): (self.kT_a, self.v_a), id(self.cb_kv): (self.kT_b, self.v_b),
                              id(self.cd_kv): (self.kT_d, self.v_d)}[id(src)]
                    P.dma(vv[k0:k0 + 128, :], self.cpl[:, 512:1024])
                    pt = self.next_pt()
                    for cc in range(4):
                        P.transpose(pt[:, cc * 128:(cc + 1) * 128], self.cpl[:, cc * 128:(cc + 1) * 128], self.identb[:])
                    self.evac(self.cpT[:], pt[:, 0:512].rearrange("p (k t) -> p k t", k=4))
                    P.dma(kT[:, k0:k0 + 128].rearrange("(c p) t -> p c t", p=128), self.cpT[:])
                elif wd == 256:
                    P.dma(self.lat_v[k0:k0 + 128, :], self.cpl[:, 0:256])
                    pt = self.next_pt()
                    for cc in range(2):
                        P.transpose(pt[:, cc * 128:(cc + 1) * 128], self.cpl[:, cc * 128:(cc + 1) * 128], self.identb[:])
                    self.evac(self.cpT[:, 0:2, :], pt[:, 0:256].rearrange("p (k t) -> p k t", k=2))
                    P.dma(self.latT[:, k0:k0 + 128].rearrange("(c p) t -> p c t", p=128), self.cpT[:, 0:2, :])
                else:
                    dst = self.krT if wd == 32 else self.kiT
                    pt = self.next_pt()
                    P.transpose(pt[0:wd, 0:128], self.cpl[:, 0:wd], self.identb[:])
                    self.evac(self.cpT[0:wd, 0, :], pt[0:wd, 0:128])
                    P.dma(dst[:, k0:k0 + 128], self.cpT[0:wd, 0, :])


def _blocks_for(self, seq, i, mixer):
    out = []
    if seq == 0:
        if mixer == "a":
            for j in range(max(0, i - 4), i + 1):
                d = i - j
                out.append((j * 128, 128, "far" if d == 3 else "near", 3 if d == 4 else d))
        elif mixer == "c":
            for j in range(i + 1):
                out.append((j * 128, 128, "near" if j == i else "far", 0))
        else:
            for j in range(i + 1):
                d = i - j
                out.append((j * 128, 128, "near" if d <= 1 else "far", d if d <= 1 else 0))
    else:
        if mixer == "a":
            for pb in range(4):
                d = 4 - pb
                out.append((pb * 128, 128, "far" if d == 3 else "near", 3 if d == 4 else d))
            out.append((A_PAST, 64, "near", 0))
        elif mixer == "c":
            for pb in range(8):
                out.append((pb * 128, 128, "far", 0))
            out.append((PAST, 64, "far", 0))
        else:
            for pb in range(8):
                out.append((pb * 128, 128, "near" if pb == 7 else "far", 1))
            out.append((PAST, 64, "near", 0))
    return out


def _next_S(self, kn, nq):
    self.s_slot = (self.s_slot + 1) % 8
    bank = self.pm[self.s_slot // 4]
    c0 = (self.s_slot % 4) * 128
    return bank[0:kn, c0:c0 + nq]


def _make_pT(self, psS, kind, bias_tile, cbias, scale, kn, nq):
    P = self.P
    self.pTi = (self.pTi + 1) % 4
    pT = self.pTs[self.pTi][0:kn, 0:nq]
    if kind == "far":
        if cbias is None:
            P.act(pT, psS, AF.Exp, scale=scale)
        else:
            P.act(pT, psS, AF.Exp, bias=cbias, scale=scale)
    else:
        self.nti = (self.nti + 1) % 2
        tmp = self.ntmp[self.nti][0:kn, 0:nq]
        P.stt(tmp, psS, scale, bias_tile, ALU.mult, ALU.add)
        P.act(pT, tmp, AF.Exp)
    return pT


def _load_kv(self, kT_src, v_src, kb, nk, vwidth, nchunk=4):
    P = self.P
    P.dma(self.kTs[:, 0:nchunk, 0:nk], kT_src[:, kb:kb + nk].rearrange("(c p) t -> p c t", p=128))
    nfull = nk // 128
    P.dma(self.vs[:, 0:nfull, 0:vwidth], v_src[kb:kb + nfull * 128, :].rearrange("(n p) f -> p n f", p=128))
    rem = nk - nfull * 128
    if rem:
        P.dma(self.vs[0:rem, nfull, 0:vwidth], v_src[kb + nfull * 128:kb + nk, :])


def _seq_info(self, seq, a=False):
    c = self.cfg
    if seq == 0:
        return 0, c.TP, 0, c.TP, 128
    kb = self.key_base(seq, a)
    nk = c.KSEQA if a else c.KSEQ
    return kb, nk, c.TP + (seq - 1) * DSEQ, DSEQ, DSEQ


def _acc_banks(self):
    self.accset ^= 1
    return (self.pm[2], self.pm[3]) if self.accset else (self.pm[4], self.pm[5])


def _phase2(self, l):
    c = self.cfg
    for seq in range(c.NSEQ):
        self.mixer_a(l, seq)
        self.mixer_b(l, seq)
        self.mixer_c(l, seq)
        self.mixer_d(l, seq)


def _mixer_a(self, l, seq):
    P = self.P
    kb, nk, tq0s, ntq, nq = self.seq_info(seq, True)
    self.load_kv(self.kT_a, self.v_a, kb, nk, 512)
    for i in range(ntq // nq):
        tq0 = tq0s + i * nq
        P.dma(self.qs[:, :, 0:nq], self.qT_a[:, tq0:tq0 + nq].rearrange("(c p) t -> p c t", p=128))
        blocks = self.blocks_for(seq, i, "a")
        for h in range(8):
            po, cc = (h % 2) * 64, h // 2
            bx, by = self.acc_banks()
            O = bx[0:64, 0:nq]
            Z = bx[0:64, 128:128 + nq]
            for bi, (k0, kn, kind, ti) in enumerate(blocks):
                S = self.next_S(kn, nq)
                P.mm(S, self.kTs[po:po + 64, cc, k0:k0 + kn], self.qs[po:po + 64, cc, 0:nq])
                pT = self.make_pT(S, kind, self.BTA[0:kn, h, ti, 0:nq], self.cb_a[0:kn, l, h:h + 1], 0.125, kn, nq)
                first, last = bi == 0, bi == len(blocks) - 1
                P.mm(O, self.vs[0:kn, k0 // 128, h * 64:(h + 1) * 64], pT, start=first, stop=last, skip=True)
                P.mm(Z, self.onesb[0:kn, 0:64], pT, start=False, stop=last, skip=True)
            rz = self.fz[0][0:64, 0:nq]
            P.recip(rz, Z)
            P.tt(self.ota[:, h, 0:nq], O, rz, ALU.mult)
        P.dma(self.OT[0:512, tq0:tq0 + nq].rearrange("(h d) t -> d h t", d=64), self.ota[:, :, 0:nq])


def _mixer_b(self, l, seq):
    P = self.P
    kb, nk, tq0s, ntq, nq = self.seq_info(seq)
    self.load_kv(self.kT_b, self.v_b, kb, nk, 512)
    for i in range(ntq // nq):
        tq0 = tq0s + i * nq
        P.dma(self.qs[:, :, 0:nq], self.qT_b[:, tq0:tq0 + nq].rearrange("(c p) t -> p c t", p=128))
        blocks = self.blocks_for(seq, i, "b")
        for h in range(4):
            bx, by = self.acc_banks()
            Oj = [bx[:, 0:nq], by[:, 0:nq]]
            Zj = [bx[:, 128:128 + nq], by[:, 128:128 + nq]]
            for bi, (k0, kn, kind, ti) in enumerate(blocks):
                first, last = bi == 0, bi == len(blocks) - 1
                for j in range(2):
                    po = j * 64
                    S = self.next_S(kn, nq)
                    P.mm(S, self.kTs[po:po + 64, h, k0:k0 + kn], self.qs[po:po + 64, h, 0:nq])
                    pT = self.make_pT(S, kind, self.BT[0:kn, h, ti, 0:nq], self.cb_t5[0:kn, h:h + 1], 0.125, kn, nq)
                    P.mm(Oj[j], self.vs[0:kn, k0 // 128, h * 128:(h + 1) * 128], pT, start=first, stop=last, skip=True)
                    P.mm(Zj[j], self.onesb[0:kn, 0:128], pT, start=False, stop=last, skip=True)
            f = self.fz
            P.recip(f[0][:, 0:nq], Zj[0])
            P.recip(f[1][:, 0:nq], Zj[1])
            P.tt(f[2][:, 0:nq], Oj[0], f[0][:, 0:nq], ALU.mult)
            P.tt(f[3][:, 0:nq], Oj[1], f[1][:, 0:nq], ALU.mult)
            o = f[4][:, 0:nq]
            P.stt(o, f[3][:, 0:nq], self.lam_t[:, 5:6], f[2][:, 0:nq], ALU.mult, ALU.add)
            sq = f[5][:, 0:nq]
            P.act(sq, o, AF.Square)
            ssb = by[:, 256:256 + nq]
            P.mm(ssb, self.onesf[:, :], sq, skip=True)
            rs = f[0][:, 0:nq]
            P.ts(rs, ssb, 1.0 / 128, EPS, ALU.mult, ALU.add)
            P.act(rs, rs, AF.Sqrt)
            P.recip(rs, rs)
            self.oti ^= 1
            ot = self.ot[self.oti][:, 0:nq]
            P.stt(ot, o, self.subg[:, 0:1], rs, ALU.mult, ALU.mult)
            P.dma(self.OT[512 + h * 128:512 + (h + 1) * 128, tq0:tq0 + nq], ot)


def _mixer_c(self, l, seq):
    P = self.P
    kb, nk, tq0s, ntq, nq = self.seq_info(seq)
    P.dma(self.kTs[:, 0:2, 0:nk], self.latT[:, kb:kb + nk].rearrange("(c p) t -> p c t", p=128))
    P.dma(self.kTs[0:32, 2, 0:nk], self.krT[:, kb:kb + nk])
    nfull = nk // 128
    P.dma(self.vs[:, 0:nfull, 0:256], self.lat_v[kb:kb + nfull * 128, :].rearrange("(n p) f -> p n f", p=128))
    if nk % 128:
        P.dma(self.vs[0:nk % 128, nfull, 0:256], self.lat_v[kb + nfull * 128:kb + nk, :])
    scale = 96.0 ** -0.5
    for i in range(ntq // nq):
        tq0 = tq0s + i * nq
        for h in range(4):
            P.dma(self.qs_c[:, h, 0:2, 0:nq], self.qT_c[h, 0:256, tq0:tq0 + nq].rearrange("(c p) t -> p c t", p=128))
            P.dma(self.qs_c[0:32, h, 2, 0:nq], self.qT_c[h, 256:288, tq0:tq0 + nq])
        blocks = self.blocks_for(seq, i, "c")
        for h in range(4):
            bx, by = self.acc_banks()
            O = [bx[:, 0:nq], bx[:, 128:128 + nq]]
            Z = by[:, 0:nq]
            for bi, (k0, kn, kind, ti) in enumerate(blocks):
                first, last = bi == 0, bi == len(blocks) - 1
                S = self.next_S(kn, nq)
                P.mm(S, self.kTs[:, 0, k0:k0 + kn], self.qs_c[:, h, 0, 0:nq], start=True, stop=False)
                P.mm(S, self.kTs[:, 1, k0:k0 + kn], self.qs_c[:, h, 1, 0:nq], start=False, stop=False)
                P.mm(S, self.kTs[0:32, 2, k0:k0 + kn], self.qs_c[0:32, h, 2, 0:nq], start=False, stop=True)
                pT = self.make_pT(S, kind, self.mt[0:kn, 0, 0:nq], None, scale, kn, nq)
                for rc in range(2):
                    P.mm(O[rc], self.vs[0:kn, k0 // 128, rc * 128:(rc + 1) * 128], pT,
                         start=(first and rc == 0), stop=last, skip=True)
                P.mm(Z, self.onesb[0:kn, 0:128], pT, start=first, stop=last, skip=True)
            rz = self.fz[0][:, 0:nq]
            P.recip(rz, Z)
            for rc in range(2):
                P.tt(self.olat[:, rc, 0:nq], O[rc], rz, ALU.mult)
            co = by[:, 128:128 + nq]
            for rc in range(2):
                P.mm(co, self.wuv[:, rc, h * 128:(h + 1) * 128], self.olat[:, rc, 0:nq], start=(rc == 0), stop=(rc == 1), skip=True)
            self.oti ^= 1
            ot = self.ot[self.oti][:, 0:nq]
            self.evac(ot, co)
            P.dma(self.OT[1024 + h * 128:1024 + (h + 1) * 128, tq0:tq0 + nq], ot)


def _mixer_d(self, l, seq):
    P = self.P
    kb, nk, tq0s, ntq, nq = self.seq_info(seq)
    self.load_kv(self.kT_d, self.v_d, kb, nk, 512)
    P.dma(self.kiT2[0:64, 0:nk], self.kiT[:, kb:kb + nk])
    P.dma(self.kiT2[64:128, 0:nk], self.kiT[:, kb:kb + nk])
    for i in range(ntq // nq):
        tq0 = tq0s + i * nq
        P.dma(self.qs[:, :, 0:nq], self.qT_d[:, tq0:tq0 + nq].rearrange("(c p) t -> p c t", p=128))
        P.dma(self.qis[:, :, 0:nq], self.qiT[:, tq0:tq0 + nq].rearrange("(c p) t -> p c t", p=128))
        P.dma(self.dws[0:nq, :], self.dw_s[tq0:tq0 + nq, :])
        nkq = (i + 1) * 128 if seq == 0 else nk
        blocks = self.blocks_for(seq, i, "d")
        use_topk = nkq > 256
        if use_topk:
            for k0 in range(0, nkq, 512):
                kw = min(512, nkq - k0)
                for hi in range(8):
                    po, cc = (hi % 2) * 64, hi // 2
                    ps = self.pm[hi % 2][0:nq, 0:kw]
                    P.mm(ps, self.qis[po:po + 64, cc, 0:nq], self.kiT2[po:po + 64, k0:k0 + kw])
                    self.iti ^= 1
                    tmp = self.itmp[self.iti][0:nq, 0:kw]
                    P.act(tmp, ps, AF.Relu, scale=0.125)
                    acc = self.sc[0:nq, k0:k0 + kw]
                    if hi == 0:
                        P.ts(acc, tmp, self.dws[0:nq, 0:1], None, ALU.mult)
                    else:
                        P.stt(acc, tmp, self.dws[0:nq, hi:hi + 1], acc, ALU.mult, ALU.add)
            if seq == 0:
                dg = self.sc[:, i * 128:(i + 1) * 128]
                P.tt(dg, dg, self.msk_s[:, :], ALU.add)
            P.copy(self.wk[0:nq, 0:nkq], self.sc[0:nq, 0:nkq], eng="pool")
            wkv = self.wk[0:nq, 0:nkq]
            m8 = self.m8[0:nq, :]
            for r in range(32):
                P.op("dve", lambda e, a=m8, b=wkv: e.max(a, b), [wkv], [m8])
                if r < 31:
                    P.op("dve", lambda e, a=m8, b=wkv: e.match_replace(b, a, b, -1e30), [wkv, m8], [wkv])
            P.ts(self.sel[0:nq, 0:nkq], self.sc[0:nq, 0:nkq], self.m8[0:nq, 7:8], None, ALU.is_ge)
            nb = (nkq + 127) // 128
            for g0 in range(0, nb, 8):
                pt = self.next_pt()
                gn = min(8, nb - g0)
                for g in range(gn):
                    k0 = (g0 + g) * 128
                    kn = min(128, nkq - k0)
                    P.transpose(pt[0:kn, g * 128:g * 128 + nq], self.sel[0:nq, k0:k0 + kn], self.identb[0:nq, 0:nq])
                if nkq - g0 * 128 >= gn * 128:
                    self.evac(self.selT[:, g0:g0 + gn, 0:nq],
                              pt[:, 0:gn * 128].rearrange("p (k t) -> p k t", k=gn)[:, :, 0:nq])
                else:
                    for g in range(gn):
                        k0 = (g0 + g) * 128
                        kn = min(128, nkq - k0)
                        self.evac(self.selT[0:kn, g0 + g, 0:nq], pt[0:kn, g * 128:g * 128 + nq])
        for h in range(8):
            po, cc = (h % 2) * 64, h // 2
            bx, by = self.acc_banks()
            O = bx[0:64, 0:nq]
            Z = bx[0:64, 128:128 + nq]
            for bi, (k0, kn, kind, ti) in enumerate(blocks):
                S = self.next_S(kn, nq)
                P.mm(S, self.kTs[po:po + 64, cc, k0:k0 + kn], self.qs[po:po + 64, cc, 0:nq])
                pT = self.make_pT(S, kind, self.BT[0:kn, 4 + h, ti, 0:nq], self.cb_t5[0:kn, 4 + h:5 + h], 0.125, kn, nq)
                if use_topk:
                    P.tt(pT, pT, self.selT[0:kn, k0 // 128, 0:nq], ALU.mult)
                first, last = bi == 0, bi == len(blocks) - 1
                P.mm(O, self.vs[0:kn, k0 // 128, h * 64:(h + 1) * 64], pT, start=first, stop=last, skip=True)
                P.mm(Z, self.onesb[0:kn, 0:64], pT, start=False, stop=last, skip=True)
            rz = self.fz[0][0:64, 0:nq]
            P.recip(rz, Z)
            P.tt(self.ota[:, h, 0:nq], O, rz, ALU.mult)
        P.dma(self.OT[1536:2048, tq0:tq0 + nq].rearrange("(h d) t -> d h t", d=64), self.ota[:, :, 0:nq])


Builder.p2_tiles = _p2_tiles
Builder.cache_prep = _cache_prep
Builder.blocks_for = _blocks_for
Builder.next_S = _next_S
Builder.make_pT = _make_pT
Builder.load_kv = _load_kv
Builder.seq_info = _seq_info
Builder.acc_banks = _acc_banks
Builder.phase2 = _phase2
Builder.mixer_a = _mixer_a
Builder.mixer_b = _mixer_b
Builder.mixer_c = _mixer_c
Builder.mixer_d = _mixer_d


def _p3_tiles(self):
    c = self.cfg
    sb = self.sb
    TS = c.TS
    NT = self.NT_S
    self.xt = sb("xt3", [128, NT, D])
    self.hT = sb("hT3", [128, NKC, TS], BF16)
    self.hb = sb("hb3", [128, D], BF16)
    self.bcA = sb("bcA3", [128, D])
    self.bcB = sb("bcB3", [128, D])
    self.bcC = sb("bcC3", [128, D])
    self.merged = sb("merged", [128, NT, D], BF16)
    self.actT = sb("actT", [128, 14, TS], BF16)
    self.OTt = sb("OTt", [128, 4, TS], BF16)
    self.wbB = [sb(f"wbB{i}", [128, 4, 512], BF16) for i in range(2)]
    self.w2b = [sb(f"w2b{i}", [128, 14, 256], BF16) for i in range(2)]
    self.sgt = [sb(f"sgt{i}", [128, 512]) for i in range(2)]
    self.sgi = 0
    self.lob = sb("lob", [128, D], BF16)
    self.loT = sb("loT", [128, NKC, 128], BF16)
    self.rtf = sb("rtf", [128, NKC, N_EXP])
    self.rth = sb("rth", [128, NKC, N_EXP], BF16)
    self.rtl = sb("rtl", [128, NKC, N_EXP], BF16)
    self.lg = sb("lg", [128, 8])
    self.rm8 = sb("rm8", [128, 8])
    self.rsm = sb("rsm", [128, 8])
    self.comb = sb("comb", [128, NT, 8])
    self.wbi = 0
    self.w2i = 0


def _next_sgt(self):
    self.sgi ^= 1
    return self.sgt[self.sgi]


def _bc_plan(self, s0):
    bufs = [self.bcA, self.bcB, self.tmpf, self.bcC]
    plan = []
    seen = {}
    for j in range(self.NT_S):
        key = tuple(self.tile_segs(s0 + j * 128))
        if key not in seen:
            seen[key] = len(seen)
        plan.append(seen[key])
    return bufs, plan


def _load_bc_plan(self, l, slot, s0):
    bufs, plan = self.bc_plan(s0)
    done = set()
    for j, bi in enumerate(plan):
        if bi in done:
            continue
        done.add(bi)
        self.load_bc(bufs[bi], l, slot, s0 + j * 128)
    return [bufs[bi] for bi in plan]


def _norm_mod3(self, l, slotA, slotS, t0, j, with_lo):
    P = self.P
    self.load_bc(self.bcA, l, slotA, t0)
    self.load_bc(self.bcB, l, slotS, t0)
    xtile = self.xt[:, j, :]
    self.rms_rstd(xtile, D, 0)
    P.stt(self.tmpf[:], xtile, self.ss[:, 0:1], self.bcA[:], ALU.mult, ALU.mult)
    P.tt(self.tmpf[:], self.tmpf[:], self.bcB[:], ALU.add)
    P.copy(self.hb[:], self.tmpf[:], eng="act")
    self.to_hT(self.hb, j)
    if with_lo:
        P.tt(self.lob[:], self.tmpf[:], self.hb[:], ALU.subtract)
        for half in range(2):
            pt = self.next_pt()
            for k in range(8):
                kc = half * 8 + k
                P.transpose(pt[:, k * 128:(k + 1) * 128], self.lob[:, kc * 128:(kc + 1) * 128], self.identb[:])
            self.evac(self.loT[:, half * 8:(half + 1) * 8, :], pt[:].rearrange("p (k t) -> p k t", k=8))


def _route(self, j):
    P = self.P
    pp = self.next_pm()
    lgp = pp[:, 0:8]
    n = 0
    for (lhs_hi, rr) in ((True, self.rth), (True, self.rtl), (False, self.rth)):
        for kc in range(NKC):
            lhsT = self.hT[:, kc, j * 128:(j + 1) * 128] if lhs_hi else self.loT[:, kc, :]
            P.mm(lgp, lhsT, rr[:, kc, :], start=(n == 0), stop=(n == 3 * NKC - 1))
            n += 1
    lg, m8, sm = self.lg, self.rm8, self.rsm
    P.copy(lg[:], lgp)
    P.op("dve", lambda e: e.max(m8[:], lg[:]), [lg[:]], [m8[:]])
    P.ts(sm[:, 0:1], m8[:, 0:1], -1.0, None, ALU.mult)
    P.act(sm[:, 2:3], m8[:, 1:2], AF.Exp, bias=sm[:, 0:1], scale=1.0)
    P.ts(sm[:, 3:4], sm[:, 2:3], 1.0, None, ALU.add)
    P.recip(sm[:, 4:5], sm[:, 3:4])
    e = self.comb[:, j, :]
    P.act(e, lg[:], AF.Exp, bias=sm[:, 0:1], scale=1.0)
    P.ts(lg[:], lg[:], m8[:, 1:2], None, ALU.is_ge)
    P.tt(e, e, lg[:], ALU.mult)
    P.ts(e, e, sm[:, 4:5], None, ALU.mult)


def _ffn_expert(self, l, s0, w1, w2, nch, gbufs, comb_col):
    c = self.cfg
    P = self.P
    TS, NT = c.TS, self.NT_S
    dff = nch * 128
    G = nch // 4
    hT = self.hT
    for q in range(4):
        ch0 = q * G
        cc = 0
        while cc < G:
            nb = min(2, G - cc)
            w = self.next_wA()
            f0 = (ch0 + cc) * 128
            self.load_w(w[:, :, 0:nb * 128], w1[:, f0:f0 + nb * 128], D, nb * 128)
            self.load_w(w[:, :, 256:256 + nb * 128], w1[:, dff + f0:dff + f0 + nb * 128], D, nb * 128)
            for b2 in range(nb):
                pg = self.next_pm()
                for kc in range(NKC):
                    P.mm(pg[:, 0:TS], w[:, kc, b2 * 128:(b2 + 1) * 128], hT[:, kc, :], start=(kc == 0), stop=(kc == NKC - 1))
                pu = self.next_pm()
                for kc in range(NKC):
                    P.mm(pu[:, 0:TS], w[:, kc, 256 + b2 * 128:256 + (b2 + 1) * 128], hT[:, kc, :], start=(kc == 0), stop=(kc == NKC - 1))
                sg = self.next_sgt()
                P.act(sg[:, 0:TS], pg[:, 0:TS], AF.Silu)
                P.tt(self.actT[:, cc + b2, :], sg[:, 0:TS], pu[:, 0:TS], ALU.mult)
            cc += nb
        for cb in range(8):
            self.w2i ^= 1
            wb = self.w2b[self.w2i]
            P.dma(wb[:, 0:G, :], w2[ch0 * 128:(ch0 + G) * 128, cb * 256:(cb + 1) * 256].rearrange("(k p) c -> p k c", p=128),
                  eng="pool")
            for j in range(NT):
                po = self.next_pm()
                for k in range(G):
                    P.mm(po[:, 0:256], self.actT[:, k, j * 128:(j + 1) * 128], wb[:, k, :], start=(k == 0), stop=(k == G - 1))
                sg = self.next_sgt()
                gsl = gbufs[j][:, cb * 256:(cb + 1) * 256]
                if comb_col is None:
                    P.tt(sg[:, 0:256], po[:, 0:256], gsl, ALU.mult)
                else:
                    P.stt(sg[:, 0:256], po[:, 0:256], self.comb[:, j, comb_col:comb_col + 1], gsl, ALU.mult, ALU.mult)
                xs = self.xt[:, j, cb * 256:(cb + 1) * 256]
                P.tt(xs, xs, sg[:, 0:256], ALU.add, eng="pool")


def _phase3(self, l):
    c = self.cfg
    P = self.P
    TS, NTOK, NT = c.TS, c.NTOK, self.NT_S
    xsrc = self.x_all if l == 0 else self.xres
    last = (l == c.depth - 1)
    moe = (l % 2 == 1)
    if moe:
        P.dma(self.rtf[:], self.moe_router[0].rearrange("(k p) e -> p k e", p=128))
        P.copy(self.rth[:], self.rtf[:])
        P.tt(self.rtl[:], self.rtf[:], self.rth[:], ALU.subtract)
    for s0 in range(0, NTOK, TS):
        for j in range(NT):
            t0 = s0 + j * 128
            P.dma(self.xt[:, j, :], xsrc[t0:t0 + 128, :])
            self.norm_mod3(l, 0, 1, t0, j, False)
        for i in range(4):
            P.dma(self.OTt[:], self.OT[i * 512:(i + 1) * 512, s0:s0 + TS].rearrange("(c p) t -> p c t", p=128))
            for cb in range(4):
                w = self.next_wA()
                self.load_w(w, self.w_gate[l, i, :, cb * 512:(cb + 1) * 512], D, 512)
                self.wbi ^= 1
                wb = self.wbB[self.wbi]
                P.dma(wb[:], self.w_branch[l, i, :, cb * 512:(cb + 1) * 512].rearrange("(k p) c -> p k c", p=128), eng="pool")
                for j in range(NT):
                    pg = self.next_pm()
                    for kc in range(NKC):
                        P.mm(pg[:], self.hT[:, kc, j * 128:(j + 1) * 128], w[:, kc, :], start=(kc == 0), stop=(kc == NKC - 1))
                    pb = self.next_pm()
                    for kc in range(4):
                        P.mm(pb[:], self.OTt[:, kc, j * 128:(j + 1) * 128], wb[:, kc, :], start=(kc == 0), stop=(kc == 3))
                    sg = self.next_sgt()
                    P.act(sg[:], pg[:], AF.Sigmoid)
                    ms = self.merged[:, j, cb * 512:(cb + 1) * 512]
                    if i == 0:
                        P.tt(ms, sg[:], pb[:], ALU.mult)
                    else:
                        P.tt(sg[:], sg[:], pb[:], ALU.mult)
                        P.tt(ms, ms, sg[:], ALU.add, eng="pool")
        for j in range(NT):
            self.to_hT(self.merged[:, j, :], j)
        gb = self.load_bc_plan(l, 2, s0)
        for cb in range(4):
            w = self.next_wA()
            self.load_w(w, self.w_out[l, :, cb * 512:(cb + 1) * 512], D, 512)
            for j in range(NT):
                po = self.next_pm()
                for kc in range(NKC):
                    P.mm(po[:], self.hT[:, kc, j * 128:(j + 1) * 128], w[:, kc, :], start=(kc == 0), stop=(kc == NKC - 1))
                sg = self.next_sgt()
                P.tt(sg[:], po[:], gb[j][:, cb * 512:(cb + 1) * 512], ALU.mult)
                xs = self.xt[:, j, cb * 512:(cb + 1) * 512]
                P.tt(xs, xs, sg[:], ALU.add, eng="pool")
        for j in range(NT):
            self.norm_mod3(l, 3, 4, s0 + j * 128, j, moe)
            if moe:
                self.route(j)
        gb = self.load_bc_plan(l, 5, s0)
        if not moe:
            self.ffn_expert(l, s0, self.ffn_w1[l // 2], self.ffn_w2[l // 2], D_FF // 128, gb, None)
        else:
            for e in range(N_EXP):
                self.ffn_expert(l, s0, self.moe_w1[l // 2, e], self.moe_w2[l // 2, e], D_FF_E // 128, gb, e)
        if last:
            P.dma(self.bcA[:], self.g_final[0:1, :].broadcast_to([128, D]))
        for j in range(NT):
            t0 = s0 + j * 128
            if last:
                self.rms_rstd(self.xt[:, j, :], D, 0)
                P.stt(self.bcB[:], self.xt[:, j, :], self.ss[:, 0:1], self.bcA[:], ALU.mult, ALU.mult)
                P.dma(self.y_all[t0:t0 + 128, :], self.bcB[:])
            else:
                P.dma(self.xres[t0:t0 + 128, :], self.xt[:, j, :])


Builder.p3_tiles = _p3_tiles
Builder.next_sgt = _next_sgt
Builder.bc_plan = _bc_plan
Builder.load_bc_plan = _load_bc_plan
Builder.norm_mod3 = _norm_mod3
Builder.route = _route
Builder.ffn_expert = _ffn_expert
Builder.phase3 = _phase3


_NC_CACHE = {}


def _core_inputs(inputs, cfg, b, consts):
    NS, L = cfg.NS, cfg.depth
    sl = slice(b * NS, (b + 1) * NS)
    f = np.ascontiguousarray
    m = {
        "x_all": f(np.concatenate([inputs["x_prompt"][b], inputs["x_sample"][sl].reshape(NS * DSEQ, D)], axis=0)),
        "c_all": f(np.concatenate([inputs["c_prompt"][b:b + 1], inputs["c_sample"][sl]], axis=0)),
        "ca_kv": f(inputs["cache_a_kv"][:L, sl].reshape(L, NS, A_PAST, 1024)),
        "cb_kv": f(inputs["cache_b_kv"][:L, sl].reshape(L, NS, PAST, 1024)),
        "cc_lat": f(inputs["cache_c_latent"][:L, sl]),
        "cc_kr": f(inputs["cache_c_krope"][:L, sl]),
        "cd_kv": f(inputs["cache_d_kv"][:L, sl].reshape(L, NS, PAST, 1024)),
        "cd_ki": f(inputs["cache_d_kidx"][:L, sl]),
        "w_ada": inputs["w_ada"][:L], "b_ada": inputs["b_ada"][:L], "g_mix": inputs["g_mix"][:L],
        "g_ffn": inputs["g_ffn"][:L], "g_final": inputs["g_final"].reshape(1, D),
        "w_in": inputs["w_in"][:L], "a_rel": inputs["a_rel_bias"][:L], "t5_bias": inputs["t5_bias"],
        "b_lambda": inputs["b_lambda"][:L].reshape(L, 256), "b_subln": inputs["b_subln"][:L],
        "c_q_norm": inputs["c_q_norm"][:L], "c_kv_norm": inputs["c_kv_norm"][:L],
        "c_w_uq": inputs["c_w_uq"][:L].reshape(L, 384, 384), "c_w_uk": inputs["c_w_uk"][:L].reshape(L, 256, 256),
        "c_w_uv": inputs["c_w_uv"][:L].reshape(L, 256, 512),
        "w_gate": inputs["w_gate"][:L], "w_branch": inputs["w_branch"][:L], "w_out": inputs["w_out"][:L],
        "ffn_w1": inputs["ffn_w1"][:1], "ffn_w2": inputs["ffn_w2"][:1],
    }
    if L > 1:
        m["moe_router"] = inputs["moe_router"][:1]
        m["moe_w1"] = inputs["moe_w1"][:1]
        m["moe_w2"] = inputs["moe_w2"][:1]
    m.update(consts)
    return {k: np.asarray(v, dtype=np.float32) for k, v in m.items()}


def run_cfg(inputs, cfg, n_cores):
    key = (cfg.TP, cfg.NS, cfg.depth, cfg.TS, cfg.phases, tuple(cfg.taps))
    if key not in _NC_CACHE:
        _NC_CACHE[key] = Builder(cfg).build()
    nc = _NC_CACHE[key]
    consts = make_consts(cfg.TP)
    in_maps = [_core_inputs(inputs, cfg, b, consts) for b in range(n_cores)]
    res = run_bass_kernel_spmd(nc, in_maps, core_ids=list(range(n_cores)))
    return res.results


def assemble(results, cfg, n_cores):
    TP, NS, L = cfg.TP, cfg.NS, cfg.depth
    B = n_cores
    y_p = np.stack([r["y_all"][:TP] for r in results])
    y_s = np.concatenate([r["y_all"][TP:].reshape(NS, DSEQ, D) for r in results])
    na = min(A_PAST, TP)

    def st(name, width, shape_tail, last=None):
        lo = TP - last if last else 0
        p = np.stack([np.asarray(r[name])[:, lo:TP] for r in results], axis=1)
        s = np.concatenate([np.asarray(r[name])[:, TP:].reshape(L, NS, DSEQ, width) for r in results], axis=1)
        return p.reshape(L, B, TP - lo, *shape_tail), s.reshape(L, B * NS, DSEQ, *shape_tail)

    pa, sa = st("st_a", 1024, (2, 8, 64), last=na)
    pb, sb_ = st("st_b", 1024, (2, 4, 128))
    pcl, scl = st("st_cl", 256, (256,))
    pck, sck = st("st_ck", 32, (32,))
    pd, sd = st("st_d", 1024, (2, 8, 64))
    pdi, sdi = st("st_di", 64, (64,))
    outs = (y_p, y_s, pa, pb, pcl, pck, pd, pdi, sa, sb_, scl, sck, sd, sdi)
    return tuple(np.ascontiguousarray(o, dtype=np.float32) for o in outs)


def kernel(**inputs):
    inputs = {k: np.asarray(v) for k, v in inputs.items()}
    B = inputs["x_prompt"].shape[0]
    cfg = Cfg(TP=inputs["x_prompt"].shape[1], NS=inputs["x_sample"].shape[0] // B,
              depth=inputs["w_in"].shape[0], TS=512)
    results = run_cfg(inputs, cfg, B)
    return assemble(results, cfg, B)
```

```python
import contextlib
import math
import numpy as np
import concourse.bass as bass
import concourse.mybir as mybir
from concourse.bass_utils import run_bass_kernel_spmd

F32 = mybir.dt.float32
BF16 = mybir.dt.bfloat16
AF = mybir.ActivationFunctionType
ALU = mybir.AluOpType
AX = mybir.AxisListType

D = 2048
NKC = 16
IN_COLS = 5864
D_FF = 5632
D_FF_E = 7168
N_EXP = 8
EPS = 1e-6
NEG = -30000.0
PAST = 1024
A_PAST = 512
DSEQ = 64


class Op:
    __slots__ = ("eng", "fn", "deps", "is_dma", "signal", "ev", "pre")

    def __init__(self, eng, fn, is_dma):
        self.eng = eng
        self.fn = fn
        self.is_dma = is_dma
        self.deps = set()
        self.signal = False
        self.ev = None
        self.pre = None


class Prog:
    ENGS = ("pe", "act", "dve", "pool", "sp")
    SEM_ROLL = 30000
    NDMA_SEMS = 24

    def __init__(self, nc):
        self.nc = nc
        self.ops = {e: [] for e in self.ENGS}
        self.acc = {}
        self.pinfo = {}
        self.nops = 0
        self.bar_pos = {}

    def _box(self, ap):
        t = ap.tensor
        name = t.name
        off = ap.offset
        if str(ap.space) == "DRAM":
            span = 0
            for st, cnt in ap.ap:
                span += abs(st) * (cnt - 1)
            return name, (0, 0, off, off + span)
        P = self.pinfo.get(name)
        if P is None:
            P = 1
            for d in t.shape[1:]:
                P *= d
            self.pinfo[name] = P
        pspan = 0
        fspan = 0
        for st, cnt in ap.ap:
            if st >= P and st % P == 0:
                pspan += (st // P) * (cnt - 1)
            else:
                fspan += abs(st) * (cnt - 1)
        p0 = off // P
        f0 = off % P
        return name, (p0, p0 + pspan, f0, f0 + fspan)

    def _track(self, op, ap, is_write):
        name, box = self._box(ap)
        lst = self.acc.get(name)
        if lst is None:
            self.acc[name] = [(box, op, is_write)]
            return
        keep = []
        b0, b1, b2, b3 = box
        for rec in lst:
            rbox, rop, rw = rec
            if not (rbox[1] < b0 or b1 < rbox[0] or rbox[3] < b2 or b3 < rbox[2]):
                if (rw or is_write) and rop is not op:
                    same = (rop.eng == op.eng and not rop.is_dma and not op.is_dma)
                    if not (same and (is_write or op.eng == "pe")):
                        op.deps.add(rop)
                if is_write and b0 <= rbox[0] and b1 >= rbox[1] and b2 <= rbox[2] and b3 >= rbox[3]:
                    continue
                if (not is_write) and (not rw) and rop.eng == op.eng and not rop.is_dma and rbox == box:
                    continue
            keep.append(rec)
        keep.append((box, op, is_write))
        self.acc[name] = keep

    def op(self, eng, fn, reads=(), writes=(), dma=False):
        o = Op(eng, fn, dma)
        for ap in reads:
            self._track(o, ap, False)
        for ap in writes:
            self._track(o, ap, True)
        self.ops[eng].append(o)
        self.nops += 1
        return o

    def barrier(self):
        lasts = []
        dmas = []
        for e in self.ENGS:
            for o in reversed(self.ops[e]):
                if not o.is_dma and o.fn is not None:
                    lasts.append(o)
                    break
            for o in self.ops[e][self.bar_pos.get(e, 0):]:
                if o.is_dma:
                    dmas.append(o)
        for e in self.ENGS:
            self.bar_pos[e] = len(self.ops[e])
        for e in self.ENGS:
            o = Op(e, None, False)
            o.deps = set(x for x in lasts if x.eng != e) | set(dmas)
            self.ops[e].append(o)
        self.acc = {}

    def dma(self, out, in_, eng="sp", **kw):
        return self.op(eng, lambda e: e.dma_start(out=out, in_=in_, **kw), [in_], [out], dma=True)

    def mm(self, out, lhsT, rhs, start=True, stop=True, skip=False):
        return self.op("pe", lambda e: e.matmul(out, lhsT, rhs, start=start, stop=stop, skip_group_check=skip),
                       [lhsT, rhs], [out])

    def transpose(self, out, in_, ident):
        return self.op("pe", lambda e: e.transpose(out, in_, ident), [in_, ident], [out])

    def act(self, out, in_, func, bias=None, scale=None, accum_out=None):
        kw = {}
        rd = [in_]
        wr = [out]
        if bias is not None:
            kw["bias"] = bias
            if not isinstance(bias, (int, float)):
                rd.append(bias)
        if scale is not None:
            kw["scale"] = scale
            if not isinstance(scale, (int, float)):
                rd.append(scale)
        if accum_out is not None:
            kw["accum_out"] = accum_out
            wr.append(accum_out)
        return self.op("act", lambda e: e.activation(out, in_, func, **kw), rd, wr)

    def ts(self, out, in0, s1, s2, op0, op1=None, eng="dve"):
        rd = [in0]
        if s1 is not None and not isinstance(s1, (int, float)):
            rd.append(s1)
        if s2 is not None and not isinstance(s2, (int, float)):
            rd.append(s2)
        kw = {}
        if op1 is not None:
            kw["op1"] = op1
        return self.op(eng, lambda e: e.tensor_scalar(out, in0, s1, s2, op0, **kw), rd, [out])

    def tt(self, out, in0, in1, op, eng="dve"):
        return self.op(eng, lambda e: e.tensor_tensor(out, in0, in1, op), [in0, in1], [out])

    def stt(self, out, in0, scalar, in1, op0, op1):
        rd = [in0, in1]
        if not isinstance(scalar, (int, float)):
            rd.append(scalar)
        return self.op("dve", lambda e: e.scalar_tensor_tensor(out, in0, scalar, in1, op0, op1), rd, [out])

    def copy(self, out, in_, eng="dve"):
        if eng == "act":
            return self.op(eng, lambda e: e.copy(out, in_), [in_], [out])
        return self.op(eng, lambda e: e.tensor_copy(out, in_), [in_], [out])

    def memset(self, ap, val, eng="pool"):
        return self.op(eng, lambda e: e.memset(ap, val), [], [ap])

    def recip(self, out, in_):
        return self.op("dve", lambda e: e.reciprocal(out, in_), [in_], [out])

    def reduce(self, out, in_, op, axis=AX.X):
        return self.op("dve", lambda e: e.tensor_reduce(out, in_, axis, op), [in_], [out])

    def emit(self):
        nc = self.nc
        engs = self.ENGS
        for e in engs:
            for o in self.ops[e]:
                for d in o.deps:
                    d.signal = True
        nsem_needed = {}
        for e in engs:
            cnt = 0
            gen = 0
            for o in self.ops[e]:
                if o.is_dma or not o.signal:
                    continue
                if cnt >= self.SEM_ROLL:
                    gen += 1
                    cnt = 0
                cnt += 1
                o.ev = ("c", e, gen, cnt)
            nsem_needed[e] = gen + 1
        dma_count = {e: 0 for e in engs}
        for e in engs:
            for o in self.ops[e]:
                if not o.is_dma:
                    continue
                i = dma_count[e]
                dma_count[e] += 1
                k = i % self.NDMA_SEMS
                r = i // self.NDMA_SEMS
                o.ev = ("d", e, k, 16 * (r + 1))
                o.pre = ("d", e, k, 16 * r) if r > 0 else None
        with contextlib.ExitStack() as st:
            sems = {}
            for e in engs:
                for g in range(nsem_needed[e]):
                    sems[("c", e, g)] = st.enter_context(nc.semaphore(f"c_{e}_{g}"))
                for k in range(min(self.NDMA_SEMS, dma_count[e])):
                    sems[("d", e, k)] = st.enter_context(nc.semaphore(f"d_{e}_{k}"))
            block = st.enter_context(nc.Block())
            starters = {"pe": block.tensor, "act": block.scalar, "dve": block.vector,
                        "pool": block.gpsimd, "sp": block.sync}

            def make_body(e):
                ops = self.ops[e]

                def body(engobj):
                    waited = {}
                    for o in ops:
                        need = {}
                        for d in o.deps:
                            ev = d.ev
                            key = ev[:3]
                            if need.get(key, 0) < ev[3]:
                                need[key] = ev[3]
                        if o.pre is not None:
                            key = o.pre[:3]
                            if need.get(key, 0) < o.pre[3]:
                                need[key] = o.pre[3]
                        for key, val in need.items():
                            if waited.get(key, 0) >= val:
                                continue
                            waited[key] = val
                            engobj.wait_ge(sems[key], val)
                        if o.fn is None:
                            continue
                        ins = o.fn(engobj)
                        if o.is_dma:
                            ins.then_inc(sems[o.ev[:3]], 16)
                        elif o.signal:
                            ins.then_inc(sems[o.ev[:3]], 1)
                    last = {}
                    for o in ops:
                        if o.is_dma:
                            last[o.ev[:3]] = o.ev[3]
                    for key, val in last.items():
                        engobj.wait_ge(sems[key], val)
                return body

            for e in engs:
                if self.ops[e]:
                    starters[e](make_body(e))


def _t5_bucket_np(rel):
    nb = 16
    max_exact = 8
    ret = np.where(rel > 0, nb, 0)
    n = np.abs(rel)
    nf = np.maximum(n, 1).astype(np.float32)
    large = max_exact + (np.log(nf / np.float32(max_exact)) / np.float32(math.log(128 / max_exact))
                         * np.float32(nb - max_exact)).astype(np.int32)
    large = np.minimum(large, nb - 1)
    return ret + np.where(n < max_exact, n, large)


def make_consts(TP):
    c = {}
    c["ident"] = np.eye(128, dtype=np.float32)
    k = np.arange(128)[:, None]
    q = np.arange(128)[None, :]
    oh = np.zeros((2, 32, 128 * 128), np.float32)
    for d in range(2):
        b = _t5_bucket_np(k - q - 128 * d).reshape(-1)
        oh[d, b, np.arange(128 * 128)] = 1.0
    c["oh_t5"] = oh
    oha = np.zeros((3, 256, 128 * 128), np.float32)
    bases = [128, 0, 0]
    for d in range(3):
        idx = (np.clip(k - q - 128 * d, -256, 256) + 256 - bases[d]).reshape(-1)
        assert idx.min() >= 0 and idx.max() < 256
        oha[d, idx, np.arange(128 * 128)] = 1.0
    c["oh_a"] = oha
    m0 = np.zeros((128, 128), np.float32)
    m0[64:, :64] = NEG
    m4 = np.zeros((128, 128), np.float32)
    m4[:64, 64:] = NEG
    c["mask_t"] = np.stack([m0, m4])
    ms = np.zeros((128, 128), np.float32)
    ms[:64, 64:] = -1e30
    c["mask_s"] = ms
    half = 16
    freqs = (10000.0 ** (-np.arange(half, dtype=np.float32) / np.float32(half))).astype(np.float32)
    pos = np.concatenate([np.arange(TP), PAST + np.arange(DSEQ)]).astype(np.float32)
    ang = (pos[:, None] * freqs[None, :]).astype(np.float32)
    c["rope_cos"] = np.cos(ang).astype(np.float32)
    c["rope_sin"] = np.sin(ang).astype(np.float32)
    return c


class Cfg:
    def __init__(self, TP=4096, NS=8, depth=2, TS=512, phases="all", taps=()):
        self.TP = TP
        self.NS = NS
        self.depth = depth
        self.TS = TS
        self.NTOK = TP + NS * DSEQ
        self.NSEQ = 1 + NS
        self.KSEQ = PAST + DSEQ
        self.KSEQA = A_PAST + DSEQ
        self.NKEY = TP + NS * self.KSEQ
        self.NKEYA = TP + NS * self.KSEQA
        self.phases = phases
        self.taps = taps
        assert TP % TS == 0 and (NS * DSEQ) % 128 == 0 and TS % 128 == 0 and self.NTOK % TS == 0


WIN_BLOCKS = [
    ("a_q", 0, 512), ("a_k", 512, 512), ("a_v", 1024, 512),
    ("b_q", 1536, 512), ("b_k", 2048, 512), ("b_v", 2560, 512),
    ("c_cq", 3072, 384), ("c_kv", 3456, 288),
    ("d_q", 3744, 512), ("d_k", 4256, 512), ("d_v", 4768, 512),
    ("d_qi", 5280, 512), ("d_kw", 5792, 72),
]


class Builder:
    def __init__(self, cfg):
        self.cfg = cfg
        self.nc = bass.Bass("TRN2", target_bir_lowering=False)
        self.P = Prog(self.nc)
        self.uid = 0

    def din(self, name, shape, dt=F32):
        return self.nc.dram_tensor(name, list(shape), dt, kind="ExternalInput").ap()

    def dout(self, name, shape, dt=F32):
        return self.nc.dram_tensor(name, list(shape), dt, kind="ExternalOutput").ap()

    def dscr(self, name, shape, dt=F32):
        return self.nc.dram_tensor(name, list(shape), dt, kind="Internal").ap()

    def sb(self, name, shape, dt=F32):
        self.uid += 1
        return self.st.enter_context(self.nc.sbuf_tensor(f"{name}_{self.uid}", list(shape), dt))

    def sbp(self, name, shape, dt=F32):
        return self.root.enter_context(self.nc.sbuf_tensor(name, list(shape), dt))

    @contextlib.contextmanager
    def scope(self):
        old = self.st
        with contextlib.ExitStack() as inner:
            self.st = inner
            yield
            self.P.barrier()
        self.st = old

    def next_pm(self):
        self.pmi = (self.pmi + 1) % len(self.pm)
        return self.pm[self.pmi]

    def next_pt(self):
        self.pti = (self.pti + 1) % 2
        return self.ptb[self.pti]

    def evac(self, out, in_):
        self.evi += 1
        return self.P.copy(out, in_, eng=("act" if self.evi % 2 else "dve"))

    def tile_segs(self, t0):
        TP = self.cfg.TP
        if t0 < TP:
            return [(0, 128, 0)]
        s = (t0 - TP) // DSEQ
        return [(0, 64, 1 + s), (64, 128, 2 + s)]

    def key_base(self, seq, a=False):
        if seq == 0:
            return 0
        return self.cfg.TP + (seq - 1) * (self.cfg.KSEQA if a else self.cfg.KSEQ)

    def tok_keyrows(self, t0, a=False):
        TP = self.cfg.TP
        if t0 < TP:
            return [(0, 128, t0)]
        s = (t0 - TP) // DSEQ
        past = A_PAST if a else PAST
        return [(0, 64, self.key_base(1 + s, a) + past), (64, 128, self.key_base(2 + s, a) + past)]

    def declare(self):
        c = self.cfg
        L, NS, NTOK, NSEQ, TP = c.depth, c.NS, c.NTOK, c.NSEQ, c.TP
        NKEY, NKEYA = c.NKEY, c.NKEYA
        d = self.din
        self.x_all = d("x_all", [NTOK, D])
        self.c_all = d("c_all", [NSEQ, D])
        self.ca_kv = d("ca_kv", [L, NS, A_PAST, 1024])
        self.cb_kv = d("cb_kv", [L, NS, PAST, 1024])
        self.cc_lat = d("cc_lat", [L, NS, PAST, 256])
        self.cc_kr = d("cc_kr", [L, NS, PAST, 32])
        self.cd_kv = d("cd_kv", [L, NS, PAST, 1024])
        self.cd_ki = d("cd_ki", [L, NS, PAST, 64])
        self.w_ada = d("w_ada", [L, D, 6 * D])
        self.b_ada = d("b_ada", [L, 6 * D])
        self.g_mix = d("g_mix", [L, D])
        self.g_ffn = d("g_ffn", [L, D])
        self.g_final = d("g_final", [1, D])
        self.w_in = d("w_in", [L, D, IN_COLS])
        self.a_rel = d("a_rel", [L, 513, 8])
        self.t5_bias = d("t5_bias", [32, 12])
        self.b_lambda = d("b_lambda", [L, 256])
        self.b_subln = d("b_subln", [L, 128])
        self.c_q_norm = d("c_q_norm", [L, 384])
        self.c_kv_norm = d("c_kv_norm", [L, 256])
        self.c_w_uq = d("c_w_uq", [L, 384, 384])
        self.c_w_uk = d("c_w_uk", [L, 256, 256])
        self.c_w_uv = d("c_w_uv", [L, 256, 512])
        self.w_gate = d("w_gate", [L, 4, D, D])
        self.w_branch = d("w_branch", [L, 4, 512, D])
        self.w_out = d("w_out", [L, D, D])
        self.ffn_w1 = d("ffn_w1", [1, D, 2 * D_FF])
        self.ffn_w2 = d("ffn_w2", [1, D_FF, D])
        if L > 1:
            self.moe_router = d("moe_router", [1, D, N_EXP])
            self.moe_w1 = d("moe_w1", [1, N_EXP, D, 2 * D_FF_E])
            self.moe_w2 = d("moe_w2", [1, N_EXP, D_FF_E, D])
        self.ident_in = d("ident", [128, 128])
        self.oh_t5 = d("oh_t5", [2, 32, 16384])
        self.oh_a = d("oh_a", [3, 256, 16384])
        self.mask_t = d("mask_t", [2, 128, 128])
        self.mask_s = d("mask_s", [128, 128])
        self.rope_cos = d("rope_cos", [TP + DSEQ, 16])
        self.rope_sin = d("rope_sin", [TP + DSEQ, 16])
        o = self.dout
        self.y_all = o("y_all", [NTOK, D])
        self.st_a = o("st_a", [L, NTOK, 1024])
        self.st_b = o("st_b", [L, NTOK, 1024])
        self.st_cl = o("st_cl", [L, NTOK, 256])
        self.st_ck = o("st_ck", [L, NTOK, 32])
        self.st_d = o("st_d", [L, NTOK, 1024])
        self.st_di = o("st_di", [L, NTOK, 64])
        s = self.dscr
        self.xres = s("xres", [NTOK, D])
        self.modv = s("modv", [L, 6, NSEQ, D])
        self.qT_a = s("qT_a", [512, NTOK], BF16)
        self.kT_a = s("kT_a", [512, NKEYA], BF16)
        self.v_a = s("v_a", [NKEYA, 512], BF16)
        self.qT_b = s("qT_b", [512, NTOK], BF16)
        self.kT_b = s("kT_b", [512, NKEY], BF16)
        self.v_b = s("v_b", [NKEY, 512], BF16)
        self.qT_c = s("qT_c", [4, 288, NTOK], BF16)
        self.latT = s("latT", [256, NKEY], BF16)
        self.krT = s("krT", [32, NKEY], BF16)
        self.lat_v = s("lat_v", [NKEY, 256], BF16)
        self.qT_d = s("qT_d", [512, NTOK], BF16)
        self.kT_d = s("kT_d", [512, NKEY], BF16)
        self.v_d = s("v_d", [NKEY, 512], BF16)
        self.qiT = s("qiT", [512, NTOK], BF16)
        self.kiT = s("kiT", [64, NKEY], BF16)
        self.dw_s = s("dw_s", [NTOK, 8])
        self.OT = s("OT", [2048, NTOK], BF16)
        self.bt_scr = s("bt_scr", [12, 2, 16384])
        self.bta_scr = s("bta_scr", [L, 8, 3, 16384])
        self.tap_out = {}
        for name in c.taps:
            src = getattr(self, name)
            self.tap_out[name] = o("tap_" + name, list(src.shape), src.dtype)

    def common_tiles(self):
        c = self.cfg
        P, sb = self.P, self.sbp
        TS = c.TS
        self.pm = [self.st.enter_context(self.nc.psum_tensor(f"pm{i}", [128, 512], F32)) for i in range(6)]
        self.ptb = [self.st.enter_context(self.nc.psum_tensor(f"ptb{i}", [128, 1024], BF16)) for i in range(2)]
        self.pmi = 0
        self.pti = 0
        self.evi = 0
        self.identf = sb("identf", [128, 128])
        self.identb = sb("identb", [128, 128], BF16)
        self.onesb = sb("onesb", [128, 128], BF16)
        self.onesf = sb("onesf", [128, 128])
        P.dma(self.identf[:], self.ident_in)
        P.copy(self.identb[:], self.identf[:])
        P.memset(self.onesb[:], 1.0)
        P.memset(self.onesf[:], 1.0)
        self.NT_S = TS // 128
        self.ss = sb("ss", [128, 8])
        self.wai = 0

    def scoped_common(self, need_tmpf=True):
        self.wA = [self.sb(f"wA{i}", [128, NKC, 512], BF16) for i in range(2)]
        if need_tmpf:
            self.tmpf = self.sb("tmpf", [128, D])

    def next_wA(self):
        self.wai = (self.wai + 1) % 2
        return self.wA[self.wai]

    def rms_rstd(self, src, width, col):
        P, ss = self.P, self.ss
        P.act(self.junk[:, 0:width], src, AF.Square, accum_out=ss[:, col:col + 1])
        P.ts(ss[:, col:col + 1], ss[:, col:col + 1], 1.0 / width, EPS, ALU.mult, ALU.add)
        P.act(ss[:, col:col + 1], ss[:, col:col + 1], AF.Sqrt)
        P.recip(ss[:, col:col + 1], ss[:, col:col + 1])

    def load_bc(self, dst, l, slot, t0):
        for (r0, r1, seq) in self.tile_segs(t0):
            self.P.dma(dst[r0:r1, :], self.modv[l, slot, seq:seq + 1, :].broadcast_to([r1 - r0, D]))

    def load_w(self, dst, src2d, rows, cols, eng="pool"):
        kc = rows // 128
        self.P.dma(dst[:, 0:kc, 0:cols], src2d.rearrange("(k p) c -> p k c", p=128), eng=eng)

    def norm_mod_to_hT(self, l, slotA, slotS, t0, j, xtile):
        P = self.P
        self.load_bc(self.bcA, l, slotA, t0)
        self.load_bc(self.bcB, l, slotS, t0)
        self.rms_rstd(xtile, D, 0)
        P.stt(self.tmpf[:], xtile, self.ss[:, 0:1], self.bcA[:], ALU.mult, ALU.mult)
        P.tt(self.hb[:], self.tmpf[:], self.bcB[:], ALU.add)
        self.to_hT(self.hb, j)

    def to_hT(self, src_bf16, j):
        P = self.P
        for half in range(2):
            pt = self.next_pt()
            for k in range(8):
                kc = half * 8 + k
                P.transpose(pt[:, k * 128:(k + 1) * 128], src_bf16[:, kc * 128:(kc + 1) * 128], self.identb[:])
            self.evac(self.hT[:, half * 8:(half + 1) * 8, j * 128:(j + 1) * 128],
                      pt[:].rearrange("p (k t) -> p k t", k=8))

    def phase0(self):
        c = self.cfg
        P, sb = self.P, self.sb
        L, NSEQ = c.depth, c.NSEQ
        csb = sb("csb", [NSEQ, D])
        csbb = sb("csbb", [NSEQ, D], BF16)
        cT = sb("cT", [128, NKC, NSEQ], BF16)
        modrow = sb("modrow", [NSEQ, 6 * D])
        grow = sb("grow", [NSEQ, D])
        P.dma(csb[:], self.c_all)
        P.act(csbb[:], csb[:], AF.Silu)
        pt = self.next_pt()
        for kc in range(NKC):
            P.transpose(pt[:, kc * 16:kc * 16 + NSEQ], csbb[:, kc * 128:(kc + 1) * 128],
                        self.identb[0:NSEQ, 0:NSEQ])
        P.copy(cT[:], pt[:, 0:NKC * 16].rearrange("p (k t) -> p k t", k=NKC)[:, :, 0:NSEQ])
        for l in range(L):
            P.dma(modrow[:], self.b_ada[l:l + 1, :].broadcast_to([NSEQ, 6 * D]))
            for cb in range(24):
                w = self.next_wA()
                self.load_w(w, self.w_ada[l, :, cb * 512:(cb + 1) * 512], D, 512)
                pp = self.next_pm()
                for kc in range(NKC):
                    P.mm(pp[0:NSEQ, :], cT[:, kc, :], w[:, kc, :], start=(kc == 0), stop=(kc == NKC - 1))
                P.tt(modrow[:, cb * 512:(cb + 1) * 512], modrow[:, cb * 512:(cb + 1) * 512], pp[0:NSEQ, :], ALU.add)
            for (gsrc, sc_i, sh_i, g_i, base) in ((self.g_mix, 1, 0, 2, 0), (self.g_ffn, 4, 3, 5, 3)):
                P.dma(grow[:], gsrc[l:l + 1, :].broadcast_to([NSEQ, D]))
                P.stt(grow[:], modrow[:, sc_i * D:(sc_i + 1) * D], 1.0, grow[:], ALU.add, ALU.mult)
                P.dma(self.modv[l, base + 0], grow[:])
                P.dma(self.modv[l, base + 1], modrow[:, sh_i * D:(sh_i + 1) * D])
                P.dma(self.modv[l, base + 2], modrow[:, g_i * D:(g_i + 1) * D])

    def bias_alloc(self):
        L = self.cfg.depth
        self.mt = self.sbp("mt", [128, 2, 128])
        self.msk_s = self.sbp("msk_s", [128, 128])
        self.cb_t5 = self.sbp("cb_t5", [128, 12])
        self.cb_a = self.sbp("cb_a", [128, L, 8])

    def bias_tiles(self):
        c = self.cfg
        P, sb = self.P, self.sb
        L = c.depth
        tabsb = sb("tabsb", [32, 12])
        ohsb = sb("ohsb", [128, 2, 512])
        rowsb = sb("rowsb", [12, 512])
        P.dma(self.mt[:], self.mask_t.rearrange("m k q -> k m q"))
        P.dma(self.msk_s[:], self.mask_s)
        P.dma(self.cb_t5[:], self.t5_bias[15:16, :].broadcast_to([128, 12]))
        P.dma(tabsb[:], self.t5_bias)
        for d in range(2):
            for ch in range(32):
                P.dma(ohsb[0:32, 0, :], self.oh_t5[d, :, ch * 512:(ch + 1) * 512])
                pp = self.next_pm()
                P.mm(pp[0:12, :], tabsb[:, :], ohsb[0:32, 0, :])
                self.evac(rowsb[:, :], pp[0:12, :])
                P.dma(self.bt_scr[:, d, ch * 512:(ch + 1) * 512], rowsb[:, :])
        atab = sb("atab", [128, 3, 8])
        for l in range(L):
            P.dma(self.cb_a[:, l, :], self.a_rel[l, 0:1, :].broadcast_to([128, 8]))
            P.dma(atab[:, :, :], self.a_rel[l, 0:384, :].rearrange("(c p) h -> p c h", p=128))
            bases = [128, 0, 0]
            for d in range(3):
                c0 = bases[d] // 128
                for ch in range(32):
                    P.dma(ohsb[:, :, :], self.oh_a[d, :, ch * 512:(ch + 1) * 512].rearrange("(c p) n -> p c n", p=128))
                    pp = self.next_pm()
                    for cc in range(2):
                        P.mm(pp[0:8, :], atab[:, c0 + cc, :], ohsb[:, cc, :], start=(cc == 0), stop=(cc == 1))
                    self.evac(rowsb[0:8, :], pp[0:8, :])
                    P.dma(self.bta_scr[l, :, d, ch * 512:(ch + 1) * 512], rowsb[0:8, :])

    def load_bias_tiles(self, l):
        P = self.P
        self.BT = self.sb("BT", [128, 12, 2, 128])
        self.BTA = self.sb("BTA", [128, 8, 4, 128])
        for h in range(12):
            for d in range(2):
                P.dma(self.BT[:, h, d, :], self.bt_scr[h, d, :].rearrange("(k q) -> k q", q=128))
            P.tt(self.BT[:, h, 0, :], self.BT[:, h, 0, :], self.mt[:, 0, :], ALU.add)
        for h in range(8):
            for d in range(3):
                P.dma(self.BTA[:, h, d, :], self.bta_scr[l, h, d, :].rearrange("(k q) -> k q", q=128))
            P.tt(self.BTA[:, h, 0, :], self.BTA[:, h, 0, :], self.mt[:, 0, :], ALU.add)
            P.ts(self.BTA[:, h, 3, :], self.mt[:, 1, :], self.cb_a[:, l, h:h + 1], None, ALU.add)

    def param_tiles(self):
        sb = self.sbp
        self.cqn_bc = sb("cqn_bc", [128, 384])
        self.ckvn_bc = sb("ckvn_bc", [128, 256])
        self.wuq = sb("wuq", [128, 3, 384], BF16)
        self.wukf = sb("wukf", [128, 2, 256])
        self.wukT = sb("wukT", [128, 4, 256], BF16)
        self.wuv = sb("wuv", [128, 2, 512], BF16)
        self.lam_t = sb("lam_t", [128, 8])
        self.lamw = sb("lamw", [128, 256])
        self.subg = sb("subg", [128, 1])
        self.cos_t = sb("cos_t", [128, 16])
        self.sin_t = sb("sin_t", [128, 16])

    def layer_params(self, l):
        P = self.P
        P.dma(self.cqn_bc[:], self.c_q_norm[l:l + 1, :].broadcast_to([128, 384]))
        P.dma(self.ckvn_bc[:], self.c_kv_norm[l:l + 1, :].broadcast_to([128, 256]))
        P.dma(self.wuq[:], self.c_w_uq[l].rearrange("(k p) c -> p k c", p=128), eng="pool")
        P.dma(self.wuv[:], self.c_w_uv[l].rearrange("(k p) c -> p k c", p=128), eng="pool")
        P.dma(self.wukf[:], self.c_w_uk[l].rearrange("(k p) c -> p k c", p=128))
        for hp in range(2):
            pp = self.next_pm()
            for cc in range(2):
                P.transpose(pp[:, cc * 128:(cc + 1) * 128], self.wukf[:, cc, hp * 128:(hp + 1) * 128], self.identf[:])
            self.evac(self.wukT[0:64, 2 * hp, :], pp[0:64, 0:256])
            self.evac(self.wukT[64:128, 2 * hp + 1, :], pp[64:128, 0:256])
        lam_init = 0.8 - 0.6 * math.exp(-0.3 * l)
        lamw, lam_t = self.lamw, self.lam_t
        P.dma(lamw[:], self.b_lambda[l:l + 1, :].broadcast_to([128, 256]))
        P.tt(lamw[:, 0:64], lamw[:, 0:64], lamw[:, 64:128], ALU.mult)
        P.tt(lamw[:, 128:192], lamw[:, 128:192], lamw[:, 192:256], ALU.mult)
        P.reduce(lam_t[:, 0:1], lamw[:, 0:64], ALU.add)
        P.reduce(lam_t[:, 1:2], lamw[:, 128:192], ALU.add)
        P.act(lam_t[:, 2:4], lam_t[:, 0:2], AF.Exp)
        P.tt(lam_t[:, 4:5], lam_t[:, 3:4], lam_t[:, 2:3], ALU.subtract)
        P.ts(lam_t[:, 5:6], lam_t[:, 4:5], -lam_init, None, ALU.add)
        P.dma(self.subg[:], self.b_subln[l:l + 1, :].rearrange("o e -> e o"))
        P.ts(self.subg[:], self.subg[:], 1.0 - lam_init, None, ALU.mult)

    def p1_tiles(self):
        c = self.cfg
        sb = self.sb
        TS = c.TS
        self.xt = sb("xt", [128, self.NT_S, D])
        self.hT = sb("hT", [128, NKC, TS], BF16)
        self.hb = sb("hb", [128, D], BF16)
        self.bcA = sb("bcA", [128, D])
        self.bcB = sb("bcB", [128, D])
        self.stg = sb("stg", [128, 512])
        self.stgb = sb("stgb", [128, 512], BF16)
        self.ftile = sb("ftile", [128, 4, TS], BF16)
        self.cqf = sb("cqf", [128, 384])
        self.cqb = sb("cqb", [128, 384], BF16)
        self.cqT = sb("cqT", [128, 3, 128], BF16)
        self.cqh = sb("cqh", [128, 384])
        self.qnb = sb("qnb", [128, 256], BF16)
        self.qrf = sb("qrf", [128, 4, 32])
        self.qrb = sb("qrb", [128, 128], BF16)
        self.qnT = sb("qnT", [128, 2, 128], BF16)
        self.qcs = sb("qcs", [128, 2, 128], BF16)
        self.rt1 = sb("rt1", [128, 4, 16])
        self.rt2 = sb("rt2", [128, 4, 16])
        self.kvf = sb("kvf", [128, 288])
        self.latf = sb("latf", [128, 256])
        self.latb = sb("latb", [128, 256], BF16)
        self.krf = sb("krf", [128, 32])
        self.krb = sb("krb", [128, 32], BF16)
        self.smallT = sb("smallT", [128, 4, 128], BF16)
        self.kwf = sb("kwf", [128, 72])
        self.kib = sb("kib", [128, 64], BF16)

    def load_rope(self, t0):
        P, TP = self.P, self.cfg.TP
        if t0 < TP:
            P.dma(self.cos_t[:], self.rope_cos[t0:t0 + 128, :])
            P.dma(self.sin_t[:], self.rope_sin[t0:t0 + 128, :])
        else:
            for r0 in (0, 64):
                P.dma(self.cos_t[r0:r0 + 64, :], self.rope_cos[TP:TP + 64, :])
                P.dma(self.sin_t[r0:r0 + 64, :], self.rope_sin[TP:TP + 64, :])

    def rope_tm(self, d1, d2, x1, x2, nh):
        P = self.P
        if nh > 1:
            cosb = self.cos_t[:].unsqueeze(1).to_broadcast([128, nh, 16])
            sinb = self.sin_t[:].unsqueeze(1).to_broadcast([128, nh, 16])
            t1 = self.rt1[:, 0:nh, :]
            t2 = self.rt2[:, 0:nh, :]
        else:
            cosb = self.cos_t[:]
            sinb = self.sin_t[:]
            t1 = self.rt1[:, 0, :]
            t2 = self.rt2[:, 0, :]
        P.tt(t1, x1, cosb, ALU.mult)
        P.tt(t2, x2, sinb, ALU.mult)
        P.tt(d1, t1, t2, ALU.subtract)
        P.tt(t1, x2, cosb, ALU.mult)
        P.tt(t2, x1, sinb, ALU.mult)
        P.tt(d2, t1, t2, ALU.add)

    def phase1(self, l):
        c = self.cfg
        P = self.P
        TS, NTOK, TP = c.TS, c.NTOK, c.TP
        NT_S = self.NT_S
        xsrc = self.x_all if l == 0 else self.xres
        hT = self.hT
        for s0 in range(0, NTOK, TS):
            for j in range(NT_S):
                t0 = s0 + j * 128
                P.dma(self.xt[:, j, :], xsrc[t0:t0 + 128, :])
                self.norm_mod_to_hT(l, 0, 1, t0, j, self.xt[:, j, :])
            for (name, c0, wd) in WIN_BLOCKS:
                w = self.next_wA()
                self.load_w(w, self.w_in[l, :, c0:c0 + wd], D, wd)
                feat = name in ("a_q", "a_k", "b_q", "b_k", "d_q", "d_k", "d_qi")
                tokm = name not in ("a_q", "b_q", "d_q", "d_qi")
                if feat:
                    for cc in range(4):
                        pp = self.next_pm()
                        for kc in range(NKC):
                            P.mm(pp[:, 0:TS], w[:, kc, cc * 128:(cc + 1) * 128], hT[:, kc, :],
                                 start=(kc == 0), stop=(kc == NKC - 1))
                        self.evac(self.ftile[:, cc, :], pp[:, 0:TS])
                    if name[2] == "q":
                        dst = {"a_q": self.qT_a, "b_q": self.qT_b, "d_q": self.qT_d, "d_qi": self.qiT}[name]
                        P.dma(dst[:, s0:s0 + TS].rearrange("(c p) t -> p c t", p=128), self.ftile[:])
                    else:
                        isa = (name == "a_k")
                        dst = {"a_k": self.kT_a, "b_k": self.kT_b, "d_k": self.kT_d}[name]
                        for j in range(NT_S):
                            for (r0, r1, k0) in self.tok_keyrows(s0 + j * 128, isa):
                                P.dma(dst[:, k0:k0 + (r1 - r0)].rearrange("(c p) t -> p c t", p=128),
                                      self.ftile[:, :, j * 128 + r0:j * 128 + r1])
                if not tokm:
                    continue
                for j in range(NT_S):
                    t0 = s0 + j * 128
                    pp = self.next_pm()
                    for kc in range(NKC):
                        P.mm(pp[:, 0:wd], hT[:, kc, j * 128:(j + 1) * 128], w[:, kc, 0:wd],
                             start=(kc == 0), stop=(kc == NKC - 1))
                    if name in ("a_k", "b_k", "d_k", "a_v", "b_v", "d_v"):
                        sto = {"a": self.st_a, "b": self.st_b, "d": self.st_d}[name[0]]
                        isv = name[2] == "v"
                        self.evac(self.stg[:], pp[:])
                        P.dma(sto[l, t0:t0 + 128, (512 if isv else 0):(1024 if isv else 512)], self.stg[:])
                        if isv:
                            self.evac(self.stgb[:], self.stg[:])
                            vdst = {"a": self.v_a, "b": self.v_b, "d": self.v_d}[name[0]]
                            for (r0, r1, k0) in self.tok_keyrows(t0, name[0] == "a"):
                                P.dma(vdst[k0:k0 + (r1 - r0), :], self.stgb[r0:r1, :])
                    elif name == "c_cq":
                        self.p1_cq(l, t0, pp)
                    elif name == "c_kv":
                        self.p1_ckv(l, t0, pp)
                    elif name == "d_kw":
                        self.p1_dkw(l, t0, pp)

    def p1_cq(self, l, t0, pp):
        P = self.P
        self.evac(self.cqf[:], pp[:, 0:384])
        self.rms_rstd(self.cqf[:], 384, 1)
        P.stt(self.cqb[:], self.cqf[:], self.ss[:, 1:2], self.cqn_bc[:], ALU.mult, ALU.mult)
        pt = self.next_pt()
        for cc in range(3):
            P.transpose(pt[:, cc * 128:(cc + 1) * 128], self.cqb[:, cc * 128:(cc + 1) * 128], self.identb[:])
        self.evac(self.cqT[:], pt[:, 0:384].rearrange("p (k t) -> p k t", k=3))
        p2 = self.next_pm()
        for cc in range(3):
            P.mm(p2[:, 0:384], self.cqT[:, cc, :], self.wuq[:, cc, :], start=(cc == 0), stop=(cc == 2))
        self.evac(self.cqh[:], p2[:, 0:384])
        cq4 = self.cqh[:].rearrange("p (h e) -> p h e", h=4)
        self.evac(self.qnb[:].rearrange("p (h n) -> p h n", h=4), cq4[:, :, 0:64])
        self.load_rope(t0)
        self.rope_tm(self.qrf[:, :, 0:16], self.qrf[:, :, 16:32], cq4[:, :, 64:80], cq4[:, :, 80:96], 4)
        self.evac(self.qrb[:], self.qrf[:].rearrange("p h e -> p (h e)"))
        pt = self.next_pt()
        for cc in range(2):
            P.transpose(pt[:, cc * 128:(cc + 1) * 128], self.qnb[:, cc * 128:(cc + 1) * 128], self.identb[:])
        P.transpose(pt[:, 256:384], self.qrb[:], self.identb[:])
        self.evac(self.qnT[:], pt[:, 0:256].rearrange("p (k t) -> p k t", k=2))
        self.evac(self.smallT[:, 0, :], pt[:, 256:384])
        for h in range(4):
            po = (h % 2) * 64
            cc = h // 2
            p3 = self.next_pm()
            for rc in range(2):
                P.mm(p3[:, rc * 128:(rc + 1) * 128], self.wukT[po:po + 64, h, rc * 128:(rc + 1) * 128],
                     self.qnT[po:po + 64, cc, :], start=True, stop=True)
            self.evac(self.qcs[:], p3[:, 0:256].rearrange("p (k t) -> p k t", k=2))
            P.dma(self.qT_c[h, 0:256, t0:t0 + 128].rearrange("(c p) t -> p c t", p=128), self.qcs[:])
            P.dma(self.qT_c[h, 256:288, t0:t0 + 128], self.smallT[h * 32:(h + 1) * 32, 0, :])

    def p1_ckv(self, l, t0, pp):
        P = self.P
        kvf = self.kvf
        self.evac(kvf[:], pp[:, 0:288])
        self.rms_rstd(kvf[:, 0:256], 256, 2)
        P.stt(self.latf[:], kvf[:, 0:256], self.ss[:, 2:3], self.ckvn_bc[:], ALU.mult, ALU.mult)
        P.dma(self.st_cl[l, t0:t0 + 128, :], self.latf[:])
        self.evac(self.latb[:], self.latf[:])
        self.load_rope(t0)
        self.rope_tm(self.krf[:, 0:16], self.krf[:, 16:32], kvf[:, 256:272], kvf[:, 272:288], 1)
        P.dma(self.st_ck[l, t0:t0 + 128, :], self.krf[:])
        self.evac(self.krb[:], self.krf[:])
        pt = self.next_pt()
        for cc in range(2):
            P.transpose(pt[:, cc * 128:(cc + 1) * 128], self.latb[:, cc * 128:(cc + 1) * 128], self.identb[:])
        P.transpose(pt[0:32, 256:384], self.krb[:], self.identb[:])
        self.evac(self.smallT[:, 1:3, :], pt[:, 0:256].rearrange("p (k t) -> p k t", k=2))
        self.evac(self.smallT[0:32, 3, :], pt[0:32, 256:384])
        for (r0, r1, k0) in self.tok_keyrows(t0):
            n = r1 - r0
            P.dma(self.lat_v[k0:k0 + n, :], self.latb[r0:r1, :])
            P.dma(self.latT[:, k0:k0 + n].rearrange("(c p) t -> p c t", p=128), self.smallT[:, 1:3, r0:r1])
            P.dma(self.krT[:, k0:k0 + n], self.smallT[0:32, 3, r0:r1])

    def p1_dkw(self, l, t0, pp):
        P = self.P
        kwf = self.kwf
        self.evac(kwf[:], pp[:, 0:72])
        P.dma(self.st_di[l, t0:t0 + 128, :], kwf[:, 0:64])
        self.evac(self.kib[:], kwf[:, 0:64])
        P.ts(kwf[:, 64:72], kwf[:, 64:72], 8.0 ** -0.5, None, ALU.mult)
        P.dma(self.dw_s[t0:t0 + 128, :], kwf[:, 64:72])
        pt = self.next_pt()
        P.transpose(pt[0:64, 0:128], self.kib[:], self.identb[:])
        self.evac(self.smallT[0:64, 0, :], pt[0:64, 0:128])
        for (r0, r1, k0) in self.tok_keyrows(t0):
            P.dma(self.kiT[:, k0:k0 + (r1 - r0)], self.smallT[0:64, 0, r0:r1])

    def emit_taps(self):
        for name, dst in self.tap_out.items():
            src = getattr(self, name)
            self.P.dma(dst, src)

    def build(self):
        c = self.cfg
        with contextlib.ExitStack() as st:
            self.st = st
            self.root = st
            self.declare()
            self.common_tiles()
            self.bias_alloc()
            self.param_tiles()
            ph = c.phases
            with self.scope():
                self.scoped_common(False)
                self.phase0()
            with self.scope():
                self.bias_tiles()
            for l in range(c.depth):
                self.layer_params(l)
                with self.scope():
                    self.scoped_common()
                    self.p1_tiles()
                    self.junk = self.hb
                    self.phase1(l)
                if ph in ("p1",):
                    continue
                with self.scope():
                    self.p2_tiles()
                    self.load_bias_tiles(l)
                    self.cache_prep(l)
                    self.phase2(l)
                if ph in ("p2",):
                    continue
                with self.scope():
                    self.scoped_common()
                    self.p3_tiles()
                    self.junk = self.hb
                    self.phase3(l)
            self.P.barrier()
            self.emit_taps()
            self.P.emit()
        return self.nc


def _p2_tiles(self):
    c = self.cfg
    sb = self.sb
    NKMAX = max(c.TP, c.KSEQ)
    NKB = (NKMAX + 127) // 128
    self.NKMAX, self.NKB = NKMAX, NKB
    self.kTs = sb("kTs", [128, 4, NKMAX], BF16)
    self.vs = sb("vs", [128, NKB, 512], BF16)
    self.kiT2 = sb("kiT2", [128, NKMAX], BF16)
    self.qs = sb("qs", [128, 4, 128], BF16)
    self.qs_c = sb("qs_c", [128, 4, 3, 128], BF16)
    self.qis = sb("qis", [128, 4, 128], BF16)
    self.dws = sb("dws", [128, 8])
    self.pTs = [sb(f"pT{i}", [128, 128], BF16) for i in range(4)]
    self.pTi = 0
    self.ntmp = [sb(f"ntmp{i}", [128, 128]) for i in range(2)]
    self.nti = 0
    self.sc = sb("sc", [128, NKMAX])
    self.wk = sb("wk", [128, NKMAX])
    self.itmp = [sb(f"itmp{i}", [128, 512]) for i in range(2)]
    self.iti = 0
    self.m8 = sb("m8", [128, 8])
    self.sel = sb("sel", [128, NKMAX], BF16)
    self.selT = sb("selT", [128, NKB, 128], BF16)
    self.fz = [sb(f"fz{i}", [128, 128]) for i in range(6)]
    self.olat = sb("olat", [128, 2, 128], BF16)
    self.ot = [sb(f"ot{i}", [128, 128], BF16) for i in range(2)]
    self.oti = 0
    self.ota = sb("ota", [64, 8, 128], BF16)
    self.cpl = sb("cpl", [128, 1024], BF16)
    self.cpT = sb("cpT", [128, 4, 128], BF16)
    self.s_slot = 0
    self.accset = 0


def _cache_prep(self, l):
    c = self.cfg
    P = self.P
    for s in range(c.NS):
        seq = 1 + s
        for (src, past, wd, a) in ((self.ca_kv, A_PAST, 1024, True), (self.cb_kv, PAST, 1024, False),
                                   (self.cd_kv, PAST, 1024, False), (self.cc_lat, PAST, 256, False),
                                   (self.cc_kr, PAST, 32, False), (self.cd_ki, PAST, 64, False)):
            kb = self.key_base(seq, a)
            for i in range(past // 128):
                P.dma(self.cpl[:, 0:wd], src[l, s, i * 128:(i + 1) * 128, :], eng="pool")
                k0 = kb + i * 128
                if wd == 1024:
                    kT, vv = {id(self.ca_kv): (self.kT_a, self.v_a), id(self.cb_kv): (self.kT_b, self.v_b),
                              id(self.cd_kv): (self.kT_d, self.v_d)}[id(src)]
                    P.dma(vv[k0:k0 + 128, :], self.cpl[:, 512:1024])
                    pt = self.next_pt()
                    for cc in range(4):
                        P.transpose(pt[:, cc * 128:(cc + 1) * 128], self.cpl[:, cc * 128:(cc + 1) * 128], self.identb[:])
                    self.evac(self.cpT[:], pt[:, 0:512].rearrange("p (k t) -> p k t", k=4))
                    P.dma(kT[:, k0:k0 + 128].rearrange("(c p) t -> p c t", p=128), self.cpT[:])
                elif wd == 256:
                    P.dma(self.lat_v[k0:k0 + 128, :], self.cpl[:, 0:256])
                    pt = self.next_pt()
                    for cc in range(2):
                        P.transpose(pt[:, cc * 128:(cc + 1) * 128], self.cpl[:, cc * 128:(cc + 1) * 128], self.identb[:])
                    self.evac(self.cpT[:, 0:2, :], pt[:, 0:256].rearrange("p (k t) -> p k t", k=2))
                    P.dma(self.latT[:, k0:k0 + 128].rearrange("(c p) t -> p c t", p=128), self.cpT[:, 0:2, :])
                else:
                    dst = self.krT if wd == 32 else self.kiT
                    pt = self.next_pt()
                    P.transpose(pt[0:wd, 0:128], self.cpl[:, 0:wd], self.identb[:])
                    self.evac(self.cpT[0:wd, 0, :], pt[0:wd, 0:128])
                    P.dma(dst[:, k0:k0 + 128], self.cpT[0:wd, 0, :])


def _blocks_for(self, seq, i, mixer):
    out = []
    if seq == 0:
        if mixer == "a":
            for j in range(max(0, i - 4), i + 1):
                d = i - j
                out.append((j * 128, 128, "far" if d == 3 else "near", 3 if d == 4 else d))
        elif mixer == "c":
            for j in range(i + 1):
                out.append((j * 128, 128, "near" if j == i else "far", 0))
        else:
            for j in range(i + 1):
                d = i - j
                out.append((j * 128, 128, "near" if d <= 1 else "far", d if d <= 1 else 0))
    else:
        if mixer == "a":
            for pb in range(4):
                d = 4 - pb
                out.append((pb * 128, 128, "far" if d == 3 else "near", 3 if d == 4 else d))
            out.append((A_PAST, 64, "near", 0))
        elif mixer == "c":
            for pb in range(8):
                out.append((pb * 128, 128, "far", 0))
            out.append((PAST, 64, "far", 0))
        else:
            for pb in range(8):
                out.append((pb * 128, 128, "near" if pb == 7 else "far", 1))
            out.append((PAST, 64, "near", 0))
    return out


def _next_S(self, kn, nq):
    self.s_slot = (self.s_slot + 1) % 8
    bank = self.pm[self.s_slot // 4]
    c0 = (self.s_slot % 4) * 128
    return bank[0:kn, c0:c0 + nq]


def _make_pT(self, psS, kind, bias_tile, cbias, scale, kn, nq):
    P = self.P
    self.pTi = (self.pTi + 1) % 4
    pT = self.pTs[self.pTi][0:kn, 0:nq]
    if kind == "far":
        if cbias is None:
            P.act(pT, psS, AF.Exp, scale=scale)
        else:
            P.act(pT, psS, AF.Exp, bias=cbias, scale=scale)
    else:
        self.nti = (self.nti + 1) % 2
        tmp = self.ntmp[self.nti][0:kn, 0:nq]
        P.stt(tmp, psS, scale, bias_tile, ALU.mult, ALU.add)
        P.act(pT, tmp, AF.Exp)
    return pT


def _load_kv(self, kT_src, v_src, kb, nk, vwidth, nchunk=4):
    P = self.P
    P.dma(self.kTs[:, 0:nchunk, 0:nk], kT_src[:, kb:kb + nk].rearrange("(c p) t -> p c t", p=128))
    nfull = nk // 128
    P.dma(self.vs[:, 0:nfull, 0:vwidth], v_src[kb:kb + nfull * 128, :].rearrange("(n p) f -> p n f", p=128))
    rem = nk - nfull * 128
    if rem:
        P.dma(self.vs[0:rem, nfull, 0:vwidth], v_src[kb + nfull * 128:kb + nk, :])


def _seq_info(self, seq, a=False):
    c = self.cfg
    if seq == 0:
        return 0, c.TP, 0, c.TP, 128
    kb = self.key_base(seq, a)
    nk = c.KSEQA if a else c.KSEQ
    return kb, nk, c.TP + (seq - 1) * DSEQ, DSEQ, DSEQ


def _acc_banks(self):
    self.accset ^= 1
    return (self.pm[2], self.pm[3]) if self.accset else (self.pm[4], self.pm[5])


def _phase2(self, l):
    c = self.cfg
    for seq in range(c.NSEQ):
        self.mixer_a(l, seq)
        self.mixer_b(l, seq)
        self.mixer_c(l, seq)
        self.mixer_d(l, seq)


def _mixer_a(self, l, seq):
    P = self.P
    kb, nk, tq0s, ntq, nq = self.seq_info(seq, True)
    self.load_kv(self.kT_a, self.v_a, kb, nk, 512)
    for i in range(ntq // nq):
        tq0 = tq0s + i * nq
        P.dma(self.qs[:, :, 0:nq], self.qT_a[:, tq0:tq0 + nq].rearrange("(c p) t -> p c t", p=128))
        blocks = self.blocks_for(seq, i, "a")
        for h in range(8):
            po, cc = (h % 2) * 64, h // 2
            bx, by = self.acc_banks()
            O = bx[0:64, 0:nq]
            Z = bx[0:64, 128:128 + nq]
            for bi, (k0, kn, kind, ti) in enumerate(blocks):
                S = self.next_S(kn, nq)
                P.mm(S, self.kTs[po:po + 64, cc, k0:k0 + kn], self.qs[po:po + 64, cc, 0:nq])
                pT = self.make_pT(S, kind, self.BTA[0:kn, h, ti, 0:nq], self.cb_a[0:kn, l, h:h + 1], 0.125, kn, nq)
                first, last = bi == 0, bi == len(blocks) - 1
                P.mm(O, self.vs[0:kn, k0 // 128, h * 64:(h + 1) * 64], pT, start=first, stop=last, skip=True)
                P.mm(Z, self.onesb[0:kn, 0:64], pT, start=False, stop=last, skip=True)
            rz = self.fz[0][0:64, 0:nq]
            P.recip(rz, Z)
            P.tt(self.ota[:, h, 0:nq], O, rz, ALU.mult)
        P.dma(self.OT[0:512, tq0:tq0 + nq].rearrange("(h d) t -> d h t", d=64), self.ota[:, :, 0:nq])


def _mixer_b(self, l, seq):
    P = self.P
    kb, nk, tq0s, ntq, nq = self.seq_info(seq)
    self.load_kv(self.kT_b, self.v_b, kb, nk, 512)
    for i in range(ntq // nq):
        tq0 = tq0s + i * nq
        P.dma(self.qs[:, :, 0:nq], self.qT_b[:, tq0:tq0 + nq].rearrange("(c p) t -> p c t", p=128))
        blocks = self.blocks_for(seq, i, "b")
        for h in range(4):
            bx, by = self.acc_banks()
            Oj = [bx[:, 0:nq], by[:, 0:nq]]
            Zj = [bx[:, 128:128 + nq], by[:, 128:128 + nq]]
            for bi, (k0, kn, kind, ti) in enumerate(blocks):
                first, last = bi == 0, bi == len(blocks) - 1
                for j in range(2):
                    po = j * 64
                    S = self.next_S(kn, nq)
                    P.mm(S, self.kTs[po:po + 64, h, k0:k0 + kn], self.qs[po:po + 64, h, 0:nq])
                    pT = self.make_pT(S, kind, self.BT[0:kn, h, ti, 0:nq], self.cb_t5[0:kn, h:h + 1], 0.125, kn, nq)
                    P.mm(Oj[j], self.vs[0:kn, k0 // 128, h * 128:(h + 1) * 128], pT, start=first, stop=last, skip=True)
                    P.mm(Zj[j], self.onesb[0:kn, 0:128], pT, start=False, stop=last, skip=True)
            f = self.fz
            P.recip(f[0][:, 0:nq], Zj[0])
            P.recip(f[1][:, 0:nq], Zj[1])
            P.tt(f[2][:, 0:nq], Oj[0], f[0][:, 0:nq], ALU.mult)
            P.tt(f[3][:, 0:nq], Oj[1], f[1][:, 0:nq], ALU.mult)
            o = f[4][:, 0:nq]
            P.stt(o, f[3][:, 0:nq], self.lam_t[:, 5:6], f[2][:, 0:nq], ALU.mult, ALU.add)
            sq = f[5][:, 0:nq]
            P.act(sq, o, AF.Square)
            ssb = by[:, 256:256 + nq]
            P.mm(ssb, self.onesf[:, :], sq, skip=True)
            rs = f[0][:, 0:nq]
            P.ts(rs, ssb, 1.0 / 128, EPS, ALU.mult, ALU.add)
            P.act(rs, rs, AF.Sqrt)
            P.recip(rs, rs)
            self.oti ^= 1
            ot = self.ot[self.oti][:, 0:nq]
            P.stt(ot, o, self.subg[:, 0:1], rs, ALU.mult, ALU.mult)
            P.dma(self.OT[512 + h * 128:512 + (h + 1) * 128, tq0:tq0 + nq], ot)


def _mixer_c(self, l, seq):
    P = self.P
    kb, nk, tq0s, ntq, nq = self.seq_info(seq)
    P.dma(self.kTs[:, 0:2, 0:nk], self.latT[:, kb:kb + nk].rearrange("(c p) t -> p c t", p=128))
    P.dma(self.kTs[0:32, 2, 0:nk], self.krT[:, kb:kb + nk])
    nfull = nk // 128
    P.dma(self.vs[:, 0:nfull, 0:256], self.lat_v[kb:kb + nfull * 128, :].rearrange("(n p) f -> p n f", p=128))
    if nk % 128:
        P.dma(self.vs[0:nk % 128, nfull, 0:256], self.lat_v[kb + nfull * 128:kb + nk, :])
    scale = 96.0 ** -0.5
    for i in range(ntq // nq):
        tq0 = tq0s + i * nq
        for h in range(4):
            P.dma(self.qs_c[:, h, 0:2, 0:nq], self.qT_c[h, 0:256, tq0:tq0 + nq].rearrange("(c p) t -> p c t", p=128))
            P.dma(self.qs_c[0:32, h, 2, 0:nq], self.qT_c[h, 256:288, tq0:tq0 + nq])
        blocks = self.blocks_for(seq, i, "c")
        for h in range(4):
            bx, by = self.acc_banks()
            O = [bx[:, 0:nq], bx[:, 128:128 + nq]]
            Z = by[:, 0:nq]
            for bi, (k0, kn, kind, ti) in enumerate(blocks):
                first, last = bi == 0, bi == len(blocks) - 1
                S = self.next_S(kn, nq)
                P.mm(S, self.kTs[:, 0, k0:k0 + kn], self.qs_c[:, h, 0, 0:nq], start=True, stop=False)
                P.mm(S, self.kTs[:, 1, k0:k0 + kn], self.qs_c[:, h, 1, 0:nq], start=False, stop=False)
                P.mm(S, self.kTs[0:32, 2, k0:k0 + kn], self.qs_c[0:32, h, 2, 0:nq], start=False, stop=True)
                pT = self.make_pT(S, kind, self.mt[0:kn, 0, 0:nq], None, scale, kn, nq)
                for rc in range(2):
                    P.mm(O[rc], self.vs[0:kn, k0 // 128, rc * 128:(rc + 1) * 128], pT,
                         start=(first and rc == 0), stop=last, skip=True)
                P.mm(Z, self.onesb[0:kn, 0:128], pT, start=first, stop=last, skip=True)
            rz = self.fz[0][:, 0:nq]
            P.recip(rz, Z)
            for rc in range(2):
                P.tt(self.olat[:, rc, 0:nq], O[rc], rz, ALU.mult)
            co = by[:, 128:128 + nq]
            for rc in range(2):
                P.mm(co, self.wuv[:, rc, h * 128:(h + 1) * 128], self.olat[:, rc, 0:nq], start=(rc == 0), stop=(rc == 1), skip=True)
            self.oti ^= 1
            ot = self.ot[self.oti][:, 0:nq]
            self.evac(ot, co)
            P.dma(self.OT[1024 + h * 128:1024 + (h + 1) * 128, tq0:tq0 + nq], ot)


def _mixer_d(self, l, seq):
    P = self.P
    kb, nk, tq0s, ntq, nq = self.seq_info(seq)
    self.load_kv(self.kT_d, self.v_d, kb, nk, 512)
    P.dma(self.kiT2[0:64, 0:nk], self.kiT[:, kb:kb + nk])
    P.dma(self.kiT2[64:128, 0:nk], self.kiT[:, kb:kb + nk])
    for i in range(ntq // nq):
        tq0 = tq0s + i * nq
        P.dma(self.qs[:, :, 0:nq], self.qT_d[:, tq0:tq0 + nq].rearrange("(c p) t -> p c t", p=128))
        P.dma(self.qis[:, :, 0:nq], self.qiT[:, tq0:tq0 + nq].rearrange("(c p) t -> p c t", p=128))
        P.dma(self.dws[0:nq, :], self.dw_s[tq0:tq0 + nq, :])
        nkq = (i + 1) * 128 if seq == 0 else nk
        blocks = self.blocks_for(seq, i, "d")
        use_topk = nkq > 256
        if use_topk:
            for k0 in range(0, nkq, 512):
                kw = min(512, nkq - k0)
                for hi in range(8):
                    po, cc = (hi % 2) * 64, hi // 2
                    ps = self.pm[hi % 2][0:nq, 0:kw]
                    P.mm(ps, self.qis[po:po + 64, cc, 0:nq], self.kiT2[po:po + 64, k0:k0 + kw])
                    self.iti ^= 1
                    tmp = self.itmp[self.iti][0:nq, 0:kw]
                    P.act(tmp, ps, AF.Relu, scale=0.125)
                    acc = self.sc[0:nq, k0:k0 + kw]
                    if hi == 0:
                        P.ts(acc, tmp, self.dws[0:nq, 0:1], None, ALU.mult)
                    else:
                        P.stt(acc, tmp, self.dws[0:nq, hi:hi + 1], acc, ALU.mult, ALU.add)
            if seq == 0:
                dg = self.sc[:, i * 128:(i + 1) * 128]
                P.tt(dg, dg, self.msk_s[:, :], ALU.add)
            P.copy(self.wk[0:nq, 0:nkq], self.sc[0:nq, 0:nkq], eng="pool")
            wkv = self.wk[0:nq, 0:nkq]
            m8 = self.m8[0:nq, :]
            for r in range(32):
                P.op("dve", lambda e, a=m8, b=wkv: e.max(a, b), [wkv], [m8])
                if r < 31:
                    P.op("dve", lambda e, a=m8, b=wkv: e.match_replace(b, a, b, -1e30), [wkv, m8], [wkv])
            P.ts(self.sel[0:nq, 0:nkq], self.sc[0:nq, 0:nkq], self.m8[0:nq, 7:8], None, ALU.is_ge)
            nb = (nkq + 127) // 128
            for g0 in range(0, nb, 8):
                pt = self.next_pt()
                gn = min(8, nb - g0)
                for g in range(gn):
                    k0 = (g0 + g) * 128
                    kn = min(128, nkq - k0)
                    P.transpose(pt[0:kn, g * 128:g * 128 + nq], self.sel[0:nq, k0:k0 + kn], self.identb[0:nq, 0:nq])
                if nkq - g0 * 128 >= gn * 128:
                    self.evac(self.selT[:, g0:g0 + gn, 0:nq],
                              pt[:, 0:gn * 128].rearrange("p (k t) -> p k t", k=gn)[:, :, 0:nq])
                else:
                    for g in range(gn):
                        k0 = (g0 + g) * 128
                        kn = min(128, nkq - k0)
                        self.evac(self.selT[0:kn, g0 + g, 0:nq], pt[0:kn, g * 128:g * 128 + nq])
        for h in range(8):
            po, cc = (h % 2) * 64, h // 2
            bx, by = self.acc_banks()
            O = bx[0:64, 0:nq]
            Z = bx[0:64, 128:128 + nq]
            for bi, (k0, kn, kind, ti) in enumerate(blocks):
                S = self.next_S(kn, nq)
                P.mm(S, self.kTs[po:po + 64, cc, k0:k0 + kn], self.qs[po:po + 64, cc, 0:nq])
                pT = self.make_pT(S, kind, self.BT[0:kn, 4 + h, ti, 0:nq], self.cb_t5[0:kn, 4 + h:5 + h], 0.125, kn, nq)
                if use_topk:
                    P.tt(pT, pT, self.selT[0:kn, k0 // 128, 0:nq], ALU.mult)
                first, last = bi == 0, bi == len(blocks) - 1
                P.mm(O, self.vs[0:kn, k0 // 128, h * 64:(h + 1) * 64], pT, start=first, stop=last, skip=True)
                P.mm(Z, self.onesb[0:kn, 0:64], pT, start=False, stop=last, skip=True)
            rz = self.fz[0][0:64, 0:nq]
            P.recip(rz, Z)
            P.tt(self.ota[:, h, 0:nq], O, rz, ALU.mult)
        P.dma(self.OT[1536:2048, tq0:tq0 + nq].rearrange("(h d) t -> d h t", d=64), self.ota[:, :, 0:nq])


Builder.p2_tiles = _p2_tiles
Builder.cache_prep = _cache_prep
Builder.blocks_for = _blocks_for
Builder.next_S = _next_S
Builder.make_pT = _make_pT
Builder.load_kv = _load_kv
Builder.seq_info = _seq_info
Builder.acc_banks = _acc_banks
Builder.phase2 = _phase2
Builder.mixer_a = _mixer_a
Builder.mixer_b = _mixer_b
Builder.mixer_c = _mixer_c
Builder.mixer_d = _mixer_d


def _p3_tiles(self):
    c = self.cfg
    sb = self.sb
    TS = c.TS
    NT = self.NT_S
    self.xt = sb("xt3", [128, NT, D])
    self.hT = sb("hT3", [128, NKC, TS], BF16)
    self.hb = sb("hb3", [128, D], BF16)
    self.bcA = sb("bcA3", [128, D])
    self.bcB = sb("bcB3", [128, D])
    self.bcC = sb("bcC3", [128, D])
    self.merged = sb("merged", [128, NT, D], BF16)
    self.actT = sb("actT", [128, 14, TS], BF16)
    self.OTt = sb("OTt", [128, 4, TS], BF16)
    self.wbB = [sb(f"wbB{i}", [128, 4, 512], BF16) for i in range(2)]
    self.w2b = [sb(f"w2b{i}", [128, 14, 256], BF16) for i in range(2)]
    self.sgt = [sb(f"sgt{i}", [128, 512]) for i in range(2)]
    self.sgi = 0
    self.lob = sb("lob", [128, D], BF16)
    self.loT = sb("loT", [128, NKC, 128], BF16)
    self.rtf = sb("rtf", [128, NKC, N_EXP])
    self.rth = sb("rth", [128, NKC, N_EXP], BF16)
    self.rtl = sb("rtl", [128, NKC, N_EXP], BF16)
    self.lg = sb("lg", [128, 8])
    self.rm8 = sb("rm8", [128, 8])
    self.rsm = sb("rsm", [128, 8])
    self.comb = sb("comb", [128, NT, 8])
    self.wbi = 0
    self.w2i = 0


def _next_sgt(self):
    self.sgi ^= 1
    return self.sgt[self.sgi]


def _bc_plan(self, s0):
    bufs = [self.bcA, self.bcB, self.tmpf, self.bcC]
    plan = []
    seen = {}
    for j in range(self.NT_S):
        key = tuple(self.tile_segs(s0 + j * 128))
        if key not in seen:
            seen[key] = len(seen)
        plan.append(seen[key])
    return bufs, plan


def _load_bc_plan(self, l, slot, s0):
    bufs, plan = self.bc_plan(s0)
    done = set()
    for j, bi in enumerate(plan):
        if bi in done:
            continue
        done.add(bi)
        self.load_bc(bufs[bi], l, slot, s0 + j * 128)
    return [bufs[bi] for bi in plan]


def _norm_mod3(self, l, slotA, slotS, t0, j, with_lo):
    P = self.P
    self.load_bc(self.bcA, l, slotA, t0)
    self.load_bc(self.bcB, l, slotS, t0)
    xtile = self.xt[:, j, :]
    self.rms_rstd(xtile, D, 0)
    P.stt(self.tmpf[:], xtile, self.ss[:, 0:1], self.bcA[:], ALU.mult, ALU.mult)
    P.tt(self.tmpf[:], self.tmpf[:], self.bcB[:], ALU.add)
    P.copy(self.hb[:], self.tmpf[:], eng="act")
    self.to_hT(self.hb, j)
    if with_lo:
        P.tt(self.lob[:], self.tmpf[:], self.hb[:], ALU.subtract)
        for half in range(2):
            pt = self.next_pt()
            for k in range(8):
                kc = half * 8 + k
                P.transpose(pt[:, k * 128:(k + 1) * 128], self.lob[:, kc * 128:(kc + 1) * 128], self.identb[:])
            self.evac(self.loT[:, half * 8:(half + 1) * 8, :], pt[:].rearrange("p (k t) -> p k t", k=8))


def _route(self, j):
    P = self.P
    pp = self.next_pm()
    lgp = pp[:, 0:8]
    n = 0
    for (lhs_hi, rr) in ((True, self.rth), (True, self.rtl), (False, self.rth)):
        for kc in range(NKC):
            lhsT = self.hT[:, kc, j * 128:(j + 1) * 128] if lhs_hi else self.loT[:, kc, :]
            P.mm(lgp, lhsT, rr[:, kc, :], start=(n == 0), stop=(n == 3 * NKC - 1))
            n += 1
    lg, m8, sm = self.lg, self.rm8, self.rsm
    P.copy(lg[:], lgp)
    P.op("dve", lambda e: e.max(m8[:], lg[:]), [lg[:]], [m8[:]])
    P.ts(sm[:, 0:1], m8[:, 0:1], -1.0, None, ALU.mult)
    P.act(sm[:, 2:3], m8[:, 1:2], AF.Exp, bias=sm[:, 0:1], scale=1.0)
    P.ts(sm[:, 3:4], sm[:, 2:3], 1.0, None, ALU.add)
    P.recip(sm[:, 4:5], sm[:, 3:4])
    e = self.comb[:, j, :]
    P.act(e, lg[:], AF.Exp, bias=sm[:, 0:1], scale=1.0)
    P.ts(lg[:], lg[:], m8[:, 1:2], None, ALU.is_ge)
    P.tt(e, e, lg[:], ALU.mult)
    P.ts(e, e, sm[:, 4:5], None, ALU.mult)


def _ffn_expert(self, l, s0, w1, w2, nch, gbufs, comb_col):
    c = self.cfg
    P = self.P
    TS, NT = c.TS, self.NT_S
    dff = nch * 128
    G = nch // 4
    hT = self.hT
    for q in range(4):
        ch0 = q * G
        cc = 0
        while cc < G:
            nb = min(2, G - cc)
            w = self.next_wA()
            f0 = (ch0 + cc) * 128
            self.load_w(w[:, :, 0:nb * 128], w1[:, f0:f0 + nb * 128], D, nb * 128)
            self.load_w(w[:, :, 256:256 + nb * 128], w1[:, dff + f0:dff + f0 + nb * 128], D, nb * 128)
            for b2 in range(nb):
                pg = self.next_pm()
                for kc in range(NKC):
                    P.mm(pg[:, 0:TS], w[:, kc, b2 * 128:(b2 + 1) * 128], hT[:, kc, :], start=(kc == 0), stop=(kc == NKC - 1))
                pu = self.next_pm()
                for kc in range(NKC):
                    P.mm(pu[:, 0:TS], w[:, kc, 256 + b2 * 128:256 + (b2 + 1) * 128], hT[:, kc, :], start=(kc == 0), stop=(kc == NKC - 1))
                sg = self.next_sgt()
                P.act(sg[:, 0:TS], pg[:, 0:TS], AF.Silu)
                P.tt(self.actT[:, cc + b2, :], sg[:, 0:TS], pu[:, 0:TS], ALU.mult)
            cc += nb
        for cb in range(8):
            self.w2i ^= 1
            wb = self.w2b[self.w2i]
            P.dma(wb[:, 0:G, :], w2[ch0 * 128:(ch0 + G) * 128, cb * 256:(cb + 1) * 256].rearrange("(k p) c -> p k c", p=128),
                  eng="pool")
            for j in range(NT):
                po = self.next_pm()
                for k in range(G):
                    P.mm(po[:, 0:256], self.actT[:, k, j * 128:(j + 1) * 128], wb[:, k, :], start=(k == 0), stop=(k == G - 1))
                sg = self.next_sgt()
                gsl = gbufs[j][:, cb * 256:(cb + 1) * 256]
                if comb_col is None:
                    P.tt(sg[:, 0:256], po[:, 0:256], gsl, ALU.mult)
                else:
                    P.stt(sg[:, 0:256], po[:, 0:256], self.comb[:, j, comb_col:comb_col + 1], gsl, ALU.mult, ALU.mult)
                xs = self.xt[:, j, cb * 256:(cb + 1) * 256]
                P.tt(xs, xs, sg[:, 0:256], ALU.add)


def _phase3(self, l):
    c = self.cfg
    P = self.P
    TS, NTOK, NT = c.TS, c.NTOK, self.NT_S
    xsrc = self.x_all if l == 0 else self.xres
    last = (l == c.depth - 1)
    moe = (l % 2 == 1)
    if moe:
        P.dma(self.rtf[:], self.moe_router[0].rearrange("(k p) e -> p k e", p=128))
        P.copy(self.rth[:], self.rtf[:])
        P.tt(self.rtl[:], self.rtf[:], self.rth[:], ALU.subtract)
    for s0 in range(0, NTOK, TS):
        for j in range(NT):
            t0 = s0 + j * 128
            P.dma(self.xt[:, j, :], xsrc[t0:t0 + 128, :])
            self.norm_mod3(l, 0, 1, t0, j, False)
        for i in range(4):
            P.dma(self.OTt[:], self.OT[i * 512:(i + 1) * 512, s0:s0 + TS].rearrange("(c p) t -> p c t", p=128))
            for cb in range(4):
                w = self.next_wA()
                self.load_w(w, self.w_gate[l, i, :, cb * 512:(cb + 1) * 512], D, 512)
                self.wbi ^= 1
                wb = self.wbB[self.wbi]
                P.dma(wb[:], self.w_branch[l, i, :, cb * 512:(cb + 1) * 512].rearrange("(k p) c -> p k c", p=128), eng="pool")
                for j in range(NT):
                    pg = self.next_pm()
                    for kc in range(NKC):
                        P.mm(pg[:], self.hT[:, kc, j * 128:(j + 1) * 128], w[:, kc, :], start=(kc == 0), stop=(kc == NKC - 1))
                    pb = self.next_pm()
                    for kc in range(4):
                        P.mm(pb[:], self.OTt[:, kc, j * 128:(j + 1) * 128], wb[:, kc, :], start=(kc == 0), stop=(kc == 3))
                    sg = self.next_sgt()
                    P.act(sg[:], pg[:], AF.Sigmoid)
                    ms = self.merged[:, j, cb * 512:(cb + 1) * 512]
                    if i == 0:
                        P.tt(ms, sg[:], pb[:], ALU.mult)
                    else:
                        P.tt(sg[:], sg[:], pb[:], ALU.mult)
                        P.tt(ms, ms, sg[:], ALU.add)
        for j in range(NT):
            self.to_hT(self.merged[:, j, :], j)
        gb = self.load_bc_plan(l, 2, s0)
        for cb in range(4):
            w = self.next_wA()
            self.load_w(w, self.w_out[l, :, cb * 512:(cb + 1) * 512], D, 512)
            for j in range(NT):
                po = self.next_pm()
                for kc in range(NKC):
                    P.mm(po[:], self.hT[:, kc, j * 128:(j + 1) * 128], w[:, kc, :], start=(kc == 0), stop=(kc == NKC - 1))
                sg = self.next_sgt()
                P.tt(sg[:], po[:], gb[j][:, cb * 512:(cb + 1) * 512], ALU.mult)
                xs = self.xt[:, j, cb * 512:(cb + 1) * 512]
                P.tt(xs, xs, sg[:], ALU.add)
        for j in range(NT):
            self.norm_mod3(l, 3, 4, s0 + j * 128, j, moe)
            if moe:
                self.route(j)
        gb = self.load_bc_plan(l, 5, s0)
        if not moe:
            self.ffn_expert(l, s0, self.ffn_w1[l // 2], self.ffn_w2[l // 2], D_FF // 128, gb, None)
        else:
            for e in range(N_EXP):
                self.ffn_expert(l, s0, self.moe_w1[l // 2, e], self.moe_w2[l // 2, e], D_FF_E // 128, gb, e)
        if last:
            P.dma(self.bcA[:], self.g_final[0:1, :].broadcast_to([128, D]))
        for j in range(NT):
            t0 = s0 + j * 128
            if last:
                self.rms_rstd(self.xt[:, j, :], D, 0)
                P.stt(self.bcB[:], self.xt[:, j, :], self.ss[:, 0:1], self.bcA[:], ALU.mult, ALU.mult)
                P.dma(self.y_all[t0:t0 + 128, :], self.bcB[:])
            else:
                P.dma(self.xres[t0:t0 + 128, :], self.xt[:, j, :])


Builder.p3_tiles = _p3_tiles
Builder.next_sgt = _next_sgt
Builder.bc_plan = _bc_plan
Builder.load_bc_plan = _load_bc_plan
Builder.norm_mod3 = _norm_mod3
Builder.route = _route
Builder.ffn_expert = _ffn_expert
Builder.phase3 = _phase3


_NC_CACHE = {}


def _core_inputs(inputs, cfg, b, consts):
    NS, L = cfg.NS, cfg.depth
    sl = slice(b * NS, (b + 1) * NS)
    f = np.ascontiguousarray
    m = {
        "x_all": f(np.concatenate([inputs["x_prompt"][b], inputs["x_sample"][sl].reshape(NS * DSEQ, D)], axis=0)),
        "c_all": f(np.concatenate([inputs["c_prompt"][b:b + 1], inputs["c_sample"][sl]], axis=0)),
        "ca_kv": f(inputs["cache_a_kv"][:L, sl].reshape(L, NS, A_PAST, 1024)),
        "cb_kv": f(inputs["cache_b_kv"][:L, sl].reshape(L, NS, PAST, 1024)),
        "cc_lat": f(inputs["cache_c_latent"][:L, sl]),
        "cc_kr": f(inputs["cache_c_krope"][:L, sl]),
        "cd_kv": f(inputs["cache_d_kv"][:L, sl].reshape(L, NS, PAST, 1024)),
        "cd_ki": f(inputs["cache_d_kidx"][:L, sl]),
        "w_ada": inputs["w_ada"][:L], "b_ada": inputs["b_ada"][:L], "g_mix": inputs["g_mix"][:L],
        "g_ffn": inputs["g_ffn"][:L], "g_final": inputs["g_final"].reshape(1, D),
        "w_in": inputs["w_in"][:L], "a_rel": inputs["a_rel_bias"][:L], "t5_bias": inputs["t5_bias"],
        "b_lambda": inputs["b_lambda"][:L].reshape(L, 256), "b_subln": inputs["b_subln"][:L],
        "c_q_norm": inputs["c_q_norm"][:L], "c_kv_norm": inputs["c_kv_norm"][:L],
        "c_w_uq": inputs["c_w_uq"][:L].reshape(L, 384, 384), "c_w_uk": inputs["c_w_uk"][:L].reshape(L, 256, 256),
        "c_w_uv": inputs["c_w_uv"][:L].reshape(L, 256, 512),
        "w_gate": inputs["w_gate"][:L], "w_branch": inputs["w_branch"][:L], "w_out": inputs["w_out"][:L],
        "ffn_w1": inputs["ffn_w1"][:1], "ffn_w2": inputs["ffn_w2"][:1],
    }
    if L > 1:
        m["moe_router"] = inputs["moe_router"][:1]
        m["moe_w1"] = inputs["moe_w1"][:1]
        m["moe_w2"] = inputs["moe_w2"][:1]
    m.update(consts)
    return {k: np.asarray(v, dtype=np.float32) for k, v in m.items()}


def run_cfg(inputs, cfg, n_cores):
    key = (cfg.TP, cfg.NS, cfg.depth, cfg.TS, cfg.phases, tuple(cfg.taps))
    if key not in _NC_CACHE:
        _NC_CACHE[key] = Builder(cfg).build()
    nc = _NC_CACHE[key]
    consts = make_consts(cfg.TP)
    in_maps = [_core_inputs(inputs, cfg, b, consts) for b in range(n_cores)]
    res = run_bass_kernel_spmd(nc, in_maps, core_ids=list(range(n_cores)))
    return res.results


def assemble(results, cfg, n_cores):
    TP, NS, L = cfg.TP, cfg.NS, cfg.depth
    B = n_cores
    y_p = np.stack([r["y_all"][:TP] for r in results])
    y_s = np.concatenate([r["y_all"][TP:].reshape(NS, DSEQ, D) for r in results])
    na = min(A_PAST, TP)

    def st(name, width, shape_tail, last=None):
        lo = TP - last if last else 0
        p = np.stack([np.asarray(r[name])[:, lo:TP] for r in results], axis=1)
        s = np.concatenate([np.asarray(r[name])[:, TP:].reshape(L, NS, DSEQ, width) for r in results], axis=1)
        return p.reshape(L, B, TP - lo, *shape_tail), s.reshape(L, B * NS, DSEQ, *shape_tail)

    pa, sa = st("st_a", 1024, (2, 8, 64), last=na)
    pb, sb_ = st("st_b", 1024, (2, 4, 128))
    pcl, scl = st("st_cl", 256, (256,))
    pck, sck = st("st_ck", 32, (32,))
    pd, sd = st("st_d", 1024, (2, 8, 64))
    pdi, sdi = st("st_di", 64, (64,))
    outs = (y_p, y_s, pa, pb, pcl, pck, pd, pdi, sa, sb_, scl, sck, sd, sdi)
    return tuple(np.ascontiguousarray(o, dtype=np.float32) for o in outs)


def kernel(**inputs):
    inputs = {k: np.asarray(v) for k, v in inputs.items()}
    B = inputs["x_prompt"].shape[0]
    cfg = Cfg(TP=inputs["x_prompt"].shape[1], NS=inputs["x_sample"].shape[0] // B,
              depth=inputs["w_in"].shape[0], TS=512)
    results = run_cfg(inputs, cfg, B)
    return assemble(results, cfg, B)
```
